# Optimizing a Trainium2 kernel written in Bass

```python
import math
import jax, jax.numpy as jnp
from jax import lax
import numpy as np

D_MODEL = 1024
BATCH = 32
SEQ = 2048
DEPTH = 4

N_EVEN = (DEPTH + 1) // 2
N_ODD = DEPTH // 2
NORM_EPS = 1e-6
CHUNK = 64
S5_WIDTH = D_MODEL // 2
S5_GROUP_SIZE = 16
S5_GROUPS = S5_WIDTH // S5_GROUP_SIZE
S5_STATE = 64
DT_MIN = 1e-3
DT_MAX = 1e-1
GLA_HEADS = 4
GLA_VW = D_MODEL // 2
GLA_KW = GLA_VW // 2
GLA_DV_HEAD = GLA_VW // GLA_HEADS
GLA_DK_HEAD = GLA_KW // GLA_HEADS
GLA_RANK = 16
GLA_GATE_TEMP = 16.0
EVEN_MIX_WIDTH = S5_WIDTH + GLA_VW
EVEN_PROJ = S5_WIDTH + 2 * GLA_KW + 2 * GLA_VW + GLA_RANK
MLSTM_INNER = 2 * D_MODEL
MLSTM_HEADS = 4
MLSTM_HEAD_DIM = MLSTM_INNER // MLSTM_HEADS
MLSTM_CONV = 4
MLSTM_QKV_BLOCK = 4
MLSTM_NBLOCKS = MLSTM_INNER // MLSTM_QKV_BLOCK
FFN_DIM = 2816
FFN_CONV = 3

kernel_name = "hybrid_s5_gla_mlstm_trunk"


def rms_norm(x, g):
    xf = x.astype(jnp.float32)
    y = xf * lax.rsqrt(jnp.mean(xf * xf, axis=-1, keepdims=True) + NORM_EPS)
    return (y * g.astype(jnp.float32)).astype(x.dtype)


def head_rms_norm(x, g, n_heads):
    b, l, w = x.shape
    xh = x.astype(jnp.float32).reshape(b, l, n_heads, w // n_heads)
    y = xh * lax.rsqrt(jnp.mean(xh * xh, axis=-1, keepdims=True) + NORM_EPS)
    return y.reshape(b, l, w) * g.astype(jnp.float32)


def causal_dwconv(x, w, b):
    width = w.shape[0]
    seq = x.shape[1]
    xp = jnp.pad(x, ((0, 0), (width - 1, 0), (0, 0)))
    out = b
    for k in range(width):
        out = out + xp[:, k:k + seq] * w[k]
    return out


def to_chunks(t):
    b, l = t.shape[:2]
    return jnp.moveaxis(t.reshape((b, l // CHUNK, CHUNK) + t.shape[2:]), 1, 0)


def from_chunks(t):
    n, b, c = t.shape[:3]
    return jnp.moveaxis(t, 0, 1).reshape((b, n * c) + t.shape[3:])


def _complex_affine_combine(e1, e2):
    a1r, a1i, b1r, b1i = e1
    a2r, a2i, b2r, b2i = e2
    return (a2r * a1r - a2i * a1i,
            a2r * a1i + a2i * a1r,
            a2r * b1r - a2i * b1i + b2r,
            a2r * b1i + a2i * b1r + b2i)


def s5_mixer(u, lam_re, lam_im, b_re, b_im, c_re, c_im, d_skip, log_dt, w_glu, b_glu):
    f32 = jnp.float32
    bsz, seq, _ = u.shape
    ug = u.astype(f32).reshape(bsz, seq, S5_GROUPS, S5_GROUP_SIZE)
    dt = jnp.exp(log_dt.astype(f32))[:, None]
    lr = lam_re.astype(f32)
    li = lam_im.astype(f32)
    mag = jnp.exp(lr * dt)
    lb_re = mag * jnp.cos(li * dt)
    lb_im = mag * jnp.sin(li * dt)
    inv = 1.0 / (lr * lr + li * li)
    zr = ((lb_re - 1.0) * lr + lb_im * li) * inv
    zi = (lb_im * lr - (lb_re - 1.0) * li) * inv
    br = b_re.astype(f32)
    bi = b_im.astype(f32)
    bb_re = zr[..., None] * br - zi[..., None] * bi
    bb_im = zr[..., None] * bi + zi[..., None] * br
    bu_re = jnp.einsum('blgi,gpi->blgp', ug, bb_re)
    bu_im = jnp.einsum('blgi,gpi->blgp', ug, bb_im)
    shape = (1, seq, S5_GROUPS, S5_STATE)
    a_re = jnp.broadcast_to(lb_re, shape)
    a_im = jnp.broadcast_to(lb_im, shape)
    _, _, x_re, x_im = lax.associative_scan(
        _complex_affine_combine, (a_re, a_im, bu_re, bu_im), axis=1)
    y = (jnp.einsum('blgp,gip->blgi', x_re, c_re.astype(f32))
         - jnp.einsum('blgp,gip->blgi', x_im, c_im.astype(f32))
         + d_skip.astype(f32) * ug)
    y = jax.nn.gelu(y.reshape(bsz, seq, S5_WIDTH), approximate=True)
    return y * jax.nn.sigmoid(y @ w_glu.astype(f32) + b_glu.astype(f32))


def gla_mixer(q, k, v, r, a_lr, w_alpha_up, b_alpha, norm_g):
    f32 = jnp.float32
    bsz, seq, _ = q.shape
    q = q.astype(f32).reshape(bsz, seq, GLA_HEADS, GLA_DK_HEAD) * (GLA_DK_HEAD ** -0.5)
    k = k.astype(f32).reshape(bsz, seq, GLA_HEADS, GLA_DK_HEAD)
    v = v.astype(f32).reshape(bsz, seq, GLA_HEADS, GLA_DV_HEAD)
    log_a = jax.nn.log_sigmoid(a_lr.astype(f32) @ w_alpha_up.astype(f32)
                               + b_alpha.astype(f32)) / GLA_GATE_TEMP
    log_a = log_a.reshape(bsz, seq, GLA_HEADS, GLA_DK_HEAD)
    qc, kc, vc = to_chunks(q), to_chunks(k), to_chunks(v)
    gc = jnp.cumsum(to_chunks(log_a), axis=2)
    mask = jnp.tril(jnp.ones((CHUNK, CHUNK), dtype=bool))

    def step(state, inp):
        qi, ki, vi, gi = inp
        g_last = gi[:, -1]
        q_dec = qi * jnp.exp(gi)
        k_dec = ki * jnp.exp(-gi)
        k_upd = ki * jnp.exp(g_last[:, None] - gi)
        scores = jnp.where(mask, jnp.einsum('bihd,bjhd->bhij', q_dec, k_dec), 0.0)
        out = (jnp.einsum('bhij,bjhv->bihv', scores, vi)
               + jnp.einsum('bihd,bhdv->bihv', q_dec, state))
        state = jnp.exp(g_last)[..., None] * state + jnp.einsum('bjhd,bjhv->bhdv', k_upd, vi)
        return state, out

    s0 = jnp.zeros((bsz, GLA_HEADS, GLA_DK_HEAD, GLA_DV_HEAD), f32)
    _, oc = lax.scan(step, s0, (qc, kc, vc, gc))
    o = from_chunks(oc).reshape(bsz, seq, GLA_VW)
    return head_rms_norm(o, norm_g, GLA_HEADS) * jax.nn.silu(r.astype(f32))


def headwise(x, w):
    b, l, _ = x.shape
    xb = x.reshape(b, l, MLSTM_NBLOCKS, MLSTM_QKV_BLOCK)
    return jnp.einsum('blnc,ncd->blnd', xb, w.astype(jnp.float32)).reshape(b, l, MLSTM_INNER)


def mlstm_mixer(x_m, o_pre, conv_w, conv_b, w_q, w_k, w_v, w_gate, b_gate, norm_g, skip):
    f32 = jnp.float32
    bsz, seq, _ = x_m.shape
    H, DH = MLSTM_HEADS, MLSTM_HEAD_DIM
    xm = x_m.astype(f32)
    xc = jax.nn.silu(causal_dwconv(xm, conv_w.astype(f32), conv_b.astype(f32)))
    q = headwise(xc, w_q)
    k = headwise(xc, w_k)
    v = headwise(xm, w_v)
    wg = w_gate.astype(f32)
    gates = (jnp.einsum('bld,dg->blg', q, wg[0]) + jnp.einsum('bld,dg->blg', k, wg[1])
             + jnp.einsum('bld,dg->blg', v, wg[2]) + b_gate.astype(f32))
    log_i = gates[..., :H]
    log_f = jax.nn.log_sigmoid(gates[..., H:])
    qh = q.reshape(bsz, seq, H, DH)
    kh = k.reshape(bsz, seq, H, DH) * (DH ** -0.5)
    vh = v.reshape(bsz, seq, H, DH)
    mask = jnp.tril(jnp.ones((CHUNK, CHUNK), dtype=bool))

    def step(carry, inp):
        c_mat, n_vec, m = carry
        qi, ki, vi, ii, fi = inp
        f_cum = jnp.cumsum(fi, axis=1).transpose(0, 2, 1)
        i_t = ii.transpose(0, 2, 1)
        d_log = jnp.where(mask, f_cum[..., :, None] - f_cum[..., None, :] + i_t[..., None, :],
                          -jnp.inf)
        inter_log = f_cum + m[..., None]
        m_loc = jnp.maximum(inter_log, jnp.max(d_log, axis=-1))
        s = jnp.einsum('bihd,bjhd->bhij', qi, ki) * jnp.exp(d_log - m_loc[..., None])
        w_inter = jnp.exp(inter_log - m_loc)
        num = (jnp.einsum('bhij,bjhe->bihe', s, vi)
               + w_inter.transpose(0, 2, 1)[..., None] * jnp.einsum('bihd,bhed->bihe', qi, c_mat))
        den = jnp.sum(s, axis=-1) + w_inter * jnp.einsum('bihd,bhd->bhi', qi, n_vec)
        denom = jnp.maximum(jnp.abs(den), jnp.exp(-m_loc)).transpose(0, 2, 1)[..., None]
        h = num / denom
        f_last = f_cum[..., -1]
        w_log = f_last[..., None] - f_cum + i_t
        m_new = jnp.maximum(f_last + m, jnp.max(w_log, axis=-1))
        w_upd = jnp.exp(w_log - m_new[..., None]).transpose(0, 2, 1)[..., None]
        decay = jnp.exp(f_last + m - m_new)
        c_mat = decay[..., None, None] * c_mat + jnp.einsum('bjhe,bjhd->bhed', vi * w_upd, ki)
        n_vec = decay[..., None] * n_vec + jnp.sum(ki * w_upd, axis=1)
        return (c_mat, n_vec, m_new), h

    init = (jnp.zeros((bsz, H, DH, DH), f32), jnp.zeros((bsz, H, DH), f32),
            jnp.zeros((bsz, H), f32))
    _, hc = lax.scan(step, init, (to_chunks(qh), to_chunks(kh), to_chunks(vh),
                                  to_chunks(log_i), to_chunks(log_f)))
    h = from_chunks(hc).reshape(bsz, seq, MLSTM_INNER)
    h = head_rms_norm(h, norm_g, H) + skip.astype(f32) * xc
    return jax.nn.sigmoid(o_pre.astype(f32)) * h


def conv_ffn(h, w_up, conv_w, conv_b, w_down):
    g, u = jnp.split(h @ w_up, 2, axis=-1)
    g = causal_dwconv(g, conv_w, conv_b)
    return ((jax.nn.gelu(g, approximate=True) * u) @ w_down).astype(h.dtype)


def setup_inputs(seed: int = 0) -> dict:
    key = jax.random.key(seed)
    ks = jax.random.split(key, 40)
    f32 = jnp.float32

    def nrm(i, shape, scale):
        return scale * jax.random.normal(ks[i], shape, f32)

    def gain(i, shape):
        return 1.0 + 0.01 * jax.random.normal(ks[i], shape, f32)

    NE, NO, G, P, GS = N_EVEN, N_ODD, S5_GROUPS, S5_STATE, S5_GROUP_SIZE
    lam_im = math.pi * jnp.arange(P, dtype=f32) + nrm(7, (NE, G, P), 0.01)
    b_gate = jnp.concatenate(
        [nrm(26, (NO, MLSTM_HEADS), 0.1),
         jnp.linspace(3.0, 6.0, MLSTM_HEADS, dtype=f32) + nrm(27, (NO, MLSTM_HEADS), 0.01)],
        axis=-1)
    return {
        "x": nrm(0, (BATCH, SEQ, D_MODEL), 1.0),
        "ln_mix_pre": gain(1, (DEPTH, D_MODEL)),
        "ln_mix_post": gain(2, (DEPTH, D_MODEL)),
        "ln_ffn_pre": gain(3, (DEPTH, D_MODEL)),
        "ln_ffn_post": gain(4, (DEPTH, D_MODEL)),
        "ev_w_in": nrm(5, (NE, D_MODEL, EVEN_PROJ), D_MODEL ** -0.5),
        "s5_lambda_re": -0.5 + nrm(6, (NE, G, P), 0.01),
        "s5_lambda_im": lam_im,
        "s5_b_re": nrm(8, (NE, G, P, GS), (2 * GS) ** -0.5),
        "s5_b_im": nrm(9, (NE, G, P, GS), (2 * GS) ** -0.5),
        "s5_c_re": nrm(10, (NE, G, GS, P), P ** -0.5),
        "s5_c_im": nrm(11, (NE, G, GS, P), P ** -0.5),
        "s5_d": nrm(12, (NE, G, GS), 1.0),
        "s5_log_dt": jax.random.uniform(ks[13], (NE, G), f32, minval=math.log(DT_MIN),
                                        maxval=math.log(DT_MAX)),
        "s5_w_glu": nrm(14, (NE, S5_WIDTH, S5_WIDTH), S5_WIDTH ** -0.5),
        "s5_b_glu": nrm(15, (NE, S5_WIDTH), 0.01),
        "gla_w_alpha_up": nrm(16, (NE, GLA_RANK, GLA_KW), GLA_RANK ** -0.5),
        "gla_b_alpha": nrm(17, (NE, GLA_KW), 0.1),
        "gla_norm": gain(18, (NE, GLA_VW)),
        "ev_w_out": nrm(19, (NE, EVEN_MIX_WIDTH, D_MODEL), EVEN_MIX_WIDTH ** -0.5),
        "od_w_in": nrm(20, (NO, D_MODEL, 2 * MLSTM_INNER), D_MODEL ** -0.5),
        "ml_conv_w": nrm(21, (NO, MLSTM_CONV, MLSTM_INNER), MLSTM_CONV ** -0.5),
        "ml_conv_b": nrm(22, (NO, MLSTM_INNER), 0.01),
        "ml_w_q": nrm(23, (NO, MLSTM_NBLOCKS, MLSTM_QKV_BLOCK, MLSTM_QKV_BLOCK), MLSTM_QKV_BLOCK ** -0.5),
        "ml_w_k": nrm(24, (NO, MLSTM_NBLOCKS, MLSTM_QKV_BLOCK, MLSTM_QKV_BLOCK), MLSTM_QKV_BLOCK ** -0.5),
        "ml_w_v": nrm(25, (NO, MLSTM_NBLOCKS, MLSTM_QKV_BLOCK, MLSTM_QKV_BLOCK), MLSTM_QKV_BLOCK ** -0.5),
        "ml_w_gate": nrm(28, (NO, 3, MLSTM_INNER, 2 * MLSTM_HEADS), (3 * MLSTM_INNER) ** -0.5),
        "ml_b_gate": b_gate,
        "ml_norm": gain(29, (NO, MLSTM_INNER)),
        "ml_skip": gain(30, (NO, MLSTM_INNER)),
        "od_w_out": nrm(31, (NO, MLSTM_INNER, D_MODEL), MLSTM_INNER ** -0.5),
        "ffn_w_up": nrm(32, (DEPTH, D_MODEL, 2 * FFN_DIM), D_MODEL ** -0.5),
        "ffn_conv_w": nrm(33, (DEPTH, FFN_CONV, FFN_DIM), FFN_CONV ** -0.5),
        "ffn_conv_b": nrm(34, (DEPTH, FFN_DIM), 0.01),
        "ffn_w_down": nrm(35, (DEPTH, FFN_DIM, D_MODEL), FFN_DIM ** -0.5),
    }


def reference(x, ln_mix_pre, ln_mix_post, ln_ffn_pre, ln_ffn_post,
              ev_w_in, s5_lambda_re, s5_lambda_im, s5_b_re, s5_b_im, s5_c_re, s5_c_im,
              s5_d, s5_log_dt, s5_w_glu, s5_b_glu, gla_w_alpha_up, gla_b_alpha, gla_norm,
              ev_w_out, od_w_in, ml_conv_w, ml_conv_b, ml_w_q, ml_w_k, ml_w_v, ml_w_gate,
              ml_b_gate, ml_norm, ml_skip, od_w_out, ffn_w_up, ffn_conv_w, ffn_conv_b,
              ffn_w_down):
    splits = [S5_WIDTH, S5_WIDTH + GLA_KW, S5_WIDTH + 2 * GLA_KW,
              S5_WIDTH + 2 * GLA_KW + GLA_VW, S5_WIDTH + 2 * GLA_KW + 2 * GLA_VW]
    for layer in range(DEPTH):
        h = rms_norm(x, ln_mix_pre[layer])
        if layer % 2 == 0:
            e = layer // 2
            p = h @ ev_w_in[e]
            u, q, k, v, r, a_lr = jnp.split(p, splits, axis=-1)
            a_out = s5_mixer(u, s5_lambda_re[e], s5_lambda_im[e], s5_b_re[e], s5_b_im[e],
                             s5_c_re[e], s5_c_im[e], s5_d[e], s5_log_dt[e], s5_w_glu[e],
                             s5_b_glu[e])
            b_out = gla_mixer(q, k, v, r, a_lr, gla_w_alpha_up[e], gla_b_alpha[e], gla_norm[e])
            mix = (jnp.concatenate([a_out, b_out], axis=-1) @ ev_w_out[e]).astype(h.dtype)
        else:
            o = layer // 2
            x_m, o_pre = jnp.split(h @ od_w_in[o], 2, axis=-1)
            c_out = mlstm_mixer(x_m, o_pre, ml_conv_w[o], ml_conv_b[o], ml_w_q[o], ml_w_k[o],
                                ml_w_v[o], ml_w_gate[o], ml_b_gate[o], ml_norm[o], ml_skip[o])
            mix = (c_out @ od_w_out[o]).astype(h.dtype)
        x = x + rms_norm(mix, ln_mix_post[layer])
        h = rms_norm(x, ln_ffn_pre[layer])
        f = conv_ffn(h, ffn_w_up[layer], ffn_conv_w[layer], ffn_conv_b[layer], ffn_w_down[layer])
        x = x + rms_norm(f, ln_ffn_post[layer])
    return x
```

```python
import numpy as np
from contextlib import ExitStack
import concourse.bass as bass
import concourse.mybir as mybir
from concourse.bass_utils import run_bass_kernel_spmd

F32 = mybir.dt.float32
BF16 = mybir.dt.bfloat16
AF = mybir.ActivationFunctionType
ALU = mybir.AluOpType

D = 1024
L = 2048
NB = 32
DEPTH = 4
FF = 2816
NHB = FF // 128
TT = 512
NTT = L // TT
EPS = 1e-6
NCORES = 8


class Res:
    __slots__ = ("name", "w", "r")

    def __init__(self, name):
        self.name = name
        self.w = None
        self.r = []


class Sched:
    EPOCH = 20000

    def __init__(self, nc, stack):
        self.nc = nc
        self.stack = stack
        self.names = ["pe", "act", "dve", "pool", "sp"]
        self.prog = {e: [] for e in self.names}
        self.cnt = {e: 0 for e in self.names}
        self.seen = {e: {} for e in self.names}
        self.sems = {}
        self.dcnt = {}
        self.self_sync = {"pe": False, "act": True, "dve": True, "pool": True, "sp": False}

    def sem(self, key):
        if key not in self.sems:
            nm = "s_" + "_".join(str(k) for k in key)
            self.sems[key] = self.stack.enter_context(self.nc.semaphore(nm))
        return self.sems[key]

    def _waits(self, eng, reads, writes):
        need = {}
        for t in reads:
            if t.w is not None:
                k, v = t.w
                need[k] = max(need.get(k, 0), v)
        for t in writes:
            if t.w is not None:
                k, v = t.w
                need[k] = max(need.get(k, 0), v)
            for (k, v) in t.r:
                need[k] = max(need.get(k, 0), v)
        waits = []
        for k, v in need.items():
            if self.seen[eng].get(k, 0) < v:
                waits.append((k, v))
                self.seen[eng][k] = v
        return waits

    def _commit(self, ev, reads, writes):
        for t in reads:
            t.r.append(ev)
            if len(t.r) > 64:
                mx = {}
                for k, v in t.r:
                    mx[k] = max(mx.get(k, 0), v)
                t.r = list(mx.items())
        for t in writes:
            t.w = ev
            t.r = []

    def op(self, eng, fn, reads=(), writes=()):
        waits = self._waits(eng, reads, writes)
        n = self.cnt[eng]
        self.cnt[eng] += 1
        key = ("e", eng, n // self.EPOCH)
        val = n % self.EPOCH + 1
        self.sem(key)
        for k, _ in waits:
            self.sem(k)
        self.prog[eng].append((waits, fn, (key, 1)))
        if not self.self_sync[eng]:
            self.seen[eng][key] = val
        self._commit((key, val), reads, writes)

    def dma(self, q, fn, reads=(), writes=(), semkey=None):
        waits = self._waits(q, reads, writes)
        key = ("d", semkey)
        self.dcnt[key] = self.dcnt.get(key, 0) + 1
        val = 16 * self.dcnt[key]
        self.sem(key)
        for k, _ in waits:
            self.sem(k)
        self.prog[q].append((waits, fn, (key, 16)))
        self._commit((key, val), reads, writes)
        return (key, val)

    def wait_all(self, eng, events):
        waits = []
        for k, v in events:
            if self.seen[eng].get(k, 0) < v:
                waits.append((k, v))
                self.seen[eng][k] = v
        self.prog[eng].append((waits, None, None))

    def replay(self, eng, e):
        for waits, fn, inc in self.prog[eng]:
            for k, v in waits:
                e.wait_ge(self.sems[k], v)
            if fn is None:
                continue
            ins = fn(e)
            if inc is not None:
                ins.then_inc(self.sems[inc[0]], inc[1])


class Builder:
    def __init__(self, nseq, layers, parts, dbg=False):
        self.nseq = nseq
        self.layers = layers
        self.parts = parts
        self.nc = bass.Bass("TRN2", target_bir_lowering=False)
        self.stack = ExitStack()
        self.S = Sched(self.nc, self.stack)
        self.res = {}
        self.drams = {}

    def sb(self, name, shape, dt=F32):
        t = self.stack.enter_context(self.nc.sbuf_tensor(name, list(shape), dt))
        return t

    def R(self, name):
        if name not in self.res:
            self.res[name] = Res(name)
        return self.res[name]

    def I(self, eng, meth, R=(), W=(), **kw):
        self.S.op(eng, (lambda e: getattr(e, meth)(**kw)), reads=R, writes=W)

    def carve_reset(self):
        self.aoff = 0

    def carve(self, shape, dt=F32):
        n = int(np.prod(shape))
        nb = n if dt == BF16 else 2 * n
        nb = (nb + 31) // 32 * 32
        a = self.arena[:, self.aoff:self.aoff + nb]
        self.aoff += nb
        assert self.aoff <= self.ARENA, (self.aoff, self.ARENA)
        if dt != BF16:
            a = a.bitcast(dt)
        a = a[:, 0:n]
        if len(shape) == 2:
            a = a.rearrange("p (a b) -> p a b", a=shape[0])
        elif len(shape) == 3:
            a = a.rearrange("p (a b c) -> p a b c", a=shape[0], b=shape[1])
        return a

    def barrier(self):
        S = self.S
        evs = []
        for eng in S.names:
            n = S.cnt[eng]
            if n > 0:
                evs.append((("e", eng, (n - 1) // S.EPOCH), (n - 1) % S.EPOCH + 1))
        for k, n in S.dcnt.items():
            evs.append((k, 16 * n))
        for eng in S.names:
            S.wait_all(eng, evs)

    @staticmethod
    def bc(ap, n, pos=None):
        lst = [list(x) for x in ap.ap]
        if pos is None:
            lst.append([0, n])
        else:
            lst.insert(1 + pos, [0, n])
        return bass.AP(ap.tensor, ap.offset, lst)

    def din(self, name, shape, dt=F32):
        h = self.nc.dram_tensor(name, list(shape), dt, kind="ExternalInput")
        self.drams[name] = h
        return h.ap()

    def dscratch(self, name, shape, dt=BF16):
        h = self.nc.dram_tensor(name, list(shape), dt, kind="Internal")
        return h.ap()

    def build(self):
        nc, S = self.nc, self.S
        nseq = self.nseq
        x_in = self.din("x", [nseq, L, D])
        y_out = nc.dram_tensor("y", [nseq, L, D], F32, kind="ExternalOutput").ap()
        ln = {k: self.din(k, [DEPTH, D]) for k in ("ln_mix_pre", "ln_mix_post", "ln_ffn_pre", "ln_ffn_post")}
        ffn_w_up = self.din("ffn_w_up", [DEPTH, D, 2 * FF])
        ffn_conv_w = self.din("ffn_conv_w", [DEPTH, 3, FF])
        ffn_conv_b = self.din("ffn_conv_b", [DEPTH, FF])
        ffn_w_down = self.din("ffn_w_down", [DEPTH, FF, D])
        consts = self.din("consts", [128, 2048])
        EV = {"ev_w_in": [2, D, 2064], "s5_lambda_re": [2, 32, 64], "s5_lambda_im": [2, 32, 64], "s5_b_re": [2, 32, 64, 16],
              "s5_b_im": [2, 32, 64, 16], "s5_c_re": [2, 32, 16, 64], "s5_c_im": [2, 32, 16, 64], "s5_d": [2, 32, 16],
              "s5_log_dt": [2, 32], "s5_w_glu": [2, 512, 512], "s5_b_glu": [2, 512], "gla_w_alpha_up": [2, 16, 256],
              "gla_b_alpha": [2, 256], "gla_norm": [2, 512], "ev_w_out": [2, D, D]}
        OD = {"od_w_in": [2, D, 4096], "ml_conv_w": [2, 4, 2048], "ml_conv_b": [2, 2048], "ml_w_q": [2, 512, 4, 4],
              "ml_w_k": [2, 512, 4, 4], "ml_w_v": [2, 512, 4, 4], "ml_w_gate": [2, 3, 2048, 8], "ml_b_gate": [2, 8],
              "ml_norm": [2, 2048], "ml_skip": [2, 2048], "od_w_out": [2, 2048, D]}
        self.inp = {}
        has_even = "mix" in self.parts and any(l % 2 == 0 for l in self.layers)
        has_odd = "mix" in self.parts and any(l % 2 == 1 for l in self.layers)
        if has_even:
            for k, shp in EV.items():
                self.inp[k] = self.din(k, shp)
        if has_odd:
            for k, shp in OD.items():
                self.inp[k] = self.din(k, shp)
        self.consts_dram = consts

        sc_up = self.dscratch("sc_up", [DEPTH, 2, NHB, 128, 8, 128])
        sc_dn = self.dscratch("sc_dn", [DEPTH, NHB, 128, 8, 128])
        self.prologue_events = []
        R_scr = self.R("scratch_w")

        used_layers = sorted(set(self.layers))
        if "ffn" in self.parts:
            for l in used_layers:
                for gu in range(2):
                    for hb in range(NHB):
                        c0 = gu * FF + hb * 128
                        src = ffn_w_up[l, :, c0:c0 + 128].rearrange("(kt p) c -> p kt c", p=128)
                        dst = sc_up[l, gu, hb]
                        ev = S.dma("pool", (lambda e, dst=dst, src=src: e.dma_start(out=dst, in_=src)),
                                   writes=[], semkey="prolog")
                        self.prologue_events.append(ev)
                src = ffn_w_down[l].rearrange("(hb p) (ob c) -> hb p ob c", p=128, c=128)
                for hb in range(NHB):
                    ev = S.dma("pool", (lambda e, dst=sc_dn[l, hb], src=src[hb]: e.dma_start(out=dst, in_=src)),
                               writes=[], semkey="prolog")
                    self.prologue_events.append(ev)
        if has_even:
            self.sc_ein = self.dscratch("sc_ein", [2, 16, 128, 8, 128])
            self.sc_eout = self.dscratch("sc_eout", [2, 8, 128, 8, 128])
            for e_ in sorted(set(l // 2 for l in used_layers if l % 2 == 0)):
                for blk in range(16):
                    src = self.inp["ev_w_in"][e_, :, blk * 128:(blk + 1) * 128].rearrange("(kt p) c -> p kt c", p=128)
                    S.dma("pool", (lambda e, dst=self.sc_ein[e_, blk], src=src: e.dma_start(out=dst, in_=src)), semkey="prolog")
                for ob in range(8):
                    src = self.inp["ev_w_out"][e_, :, ob * 128:(ob + 1) * 128].rearrange("(kt p) c -> p kt c", p=128)
                    S.dma("pool", (lambda e, dst=self.sc_eout[e_, ob], src=src: e.dma_start(out=dst, in_=src)), semkey="prolog")
        if has_odd:
            self.prologue_odd(used_layers)
        R_scr.w = (("d", "prolog"), 16 * S.dcnt.get(("d", "prolog"), 0)) if S.dcnt.get(("d", "prolog"), 0) else None

        self.x_res = self.sb("x_res", [128, 8, L], F32)
        self.R_x = [self.R(f"x_{tt}") for tt in range(NTT)]
        self.hn = self.sb("hn", [128, 8, TT], BF16)
        self.sq = [self.sb(f"sq{i}", [128, TT], BF16) for i in range(2)]
        self.rstd = self.sb("rstd", [128, TT], F32)
        self.fbuf = self.sb("fbuf", [128, 8, TT], F32)
        self.tmpf = [self.sb(f"tmpf{i}", [128, TT], F32) for i in range(2)]
        self.cst = self.sb("cst", [128, 2048], F32)
        self.ones_bf = self.sb("ones_bf", [128, 128], BF16)
        self.lnw = {k: self.sb("w_" + k, [128, DEPTH, 8], F32) for k in ln}
        self.psum = [self.stack.enter_context(nc.psum_tensor(f"ps{i}", [128, 512], F32)) for i in range(8)]
        self.R_ps = [self.R(f"ps{i}") for i in range(8)]
        self.NSLOT = 6
        self.wslot = [self.sb(f"wslot{i}", [128, 8, 128], BF16) for i in range(self.NSLOT)]
        self.R_wslot = [self.R(f"wslot{i}") for i in range(self.NSLOT)]
        self.slot_i = 0

        self.ARENA = 45440
        self.arena = self.sb("arena", [128, self.ARENA], BF16)
        self.R_hid = [self.R(f"hid{i}") for i in range(NHB)]
        self.fcw = self.sb("fcw", [128, DEPTH, 3, NHB], F32)
        self.fcb = self.sb("fcb", [128, DEPTH, NHB], F32)

        R_c = self.R("consts")
        S.dma("sp", lambda e: e.dma_start(out=self.cst[:], in_=consts[:, :]), semkey="setup")
        for k in ln:
            S.dma("sp", (lambda e, k=k: e.dma_start(out=self.lnw[k][:], in_=ln[k].rearrange("l (t p) -> p l t", p=128),
                                                    allow_slow_non_contiguous=True)),
                  semkey="setup")
        S.dma("sp", lambda e: e.dma_start(out=self.fcw[:], in_=ffn_conv_w.rearrange("l k (t p) -> p l k t", p=128),
                                          allow_slow_non_contiguous=True), semkey="setup")
        S.dma("sp", lambda e: e.dma_start(out=self.fcb[:], in_=ffn_conv_b.rearrange("l (t p) -> p l t", p=128),
                                          allow_slow_non_contiguous=True), semkey="setup")
        self.R_c = R_c
        self.setup_extra()
        R_c.w = (("d", "setup"), 16 * S.dcnt[("d", "setup")])
        S.op("dve", lambda e: e.memset(self.ones_bf[:], 1.0 / D), writes=[self.R("ones")])

        out_events = []
        for s in range(nseq):
            self.load_x(x_in, s)
            for l in self.layers:
                if "mix" in self.parts:
                    if l % 2 == 0:
                        self.mix_even(l)
                    else:
                        self.mix_odd(l)
                if "ffn" in self.parts:
                    self.ffn_layer(l, sc_up, sc_dn, R_scr)
            out_events += self.store_x(y_out, s)
        S.wait_all("sp", out_events)

        with nc.Block() as block:
            @block.tensor
            def _(e):
                S.replay("pe", e)

            @block.scalar
            def _(e):
                S.replay("act", e)

            @block.vector
            def _(e):
                S.replay("dve", e)

            @block.gpsimd
            def _(e):
                S.replay("pool", e)

            @block.sync
            def _(e):
                S.replay("sp", e)
        self.stack.close()
        return nc

    def load_x(self, x_in, s):
        S = self.S
        ident = self.cst[:, 0:128]
        for tt in range(NTT):
            for sub in range(TT // 128):
                t0 = tt * TT + sub * 128
                stg = self.tmpf
                R_stage = self.R("fbuf")
                S.dma("sp", (lambda e, t0=t0: e.dma_start(out=self.fbuf[:, 0:2, :].rearrange("p a b -> p (a b)"),
                                                          in_=x_in[s, t0:t0 + 128, :])),
                      writes=[R_stage], semkey="xstage")
                for half in range(2):
                    pb = self.psum[half]
                    Rp = self.R_ps[half]
                    for j in range(4):
                        ft = half * 4 + j
                        S.op("pe", (lambda e, ft=ft, j=j, pb=pb: e.transpose(
                            out=pb[:, j * 128:(j + 1) * 128],
                            in_=self.fbuf[:, 0:2, :].rearrange("p a b -> p (a b)")[:, ft * 128:(ft + 1) * 128],
                            identity=ident)), reads=[R_stage, self.R_c], writes=[Rp])
                    eng = "act" if half == 0 else "dve"
                    dst = self.x_res[:, half * 4:(half + 1) * 4, t0:t0 + 128]
                    src = pb[:, :].rearrange("p (j t) -> p j t", j=4)
                    if eng == "act":
                        S.op("act", (lambda e, dst=dst, src=src: e.activation(out=dst, in_=src, func=AF.Copy)),
                             reads=[Rp], writes=[self.R_x[tt]])
                    else:
                        S.op("dve", (lambda e, dst=dst, src=src: e.tensor_copy(out=dst, in_=src)),
                             reads=[Rp], writes=[self.R_x[tt]])

    def store_x(self, y_out, s):
        S = self.S
        ident = self.cst[:, 0:128]
        evs = []
        for tt in range(NTT):
            for sub in range(TT // 128):
                t0 = tt * TT + sub * 128
                R_stage = self.R("fbuf")
                for half in range(2):
                    pb = self.psum[half]
                    Rp = self.R_ps[half]
                    for j in range(4):
                        ft = half * 4 + j
                        S.op("pe", (lambda e, ft=ft, j=j, pb=pb, t0=t0: e.transpose(
                            out=pb[:, j * 128:(j + 1) * 128], in_=self.x_res[:, ft, t0:t0 + 128], identity=ident)),
                             reads=[self.R_x[tt], self.R_c], writes=[Rp])
                    dst = self.fbuf[:, 2 + half, :]
                    if half == 0:
                        S.op("act", (lambda e, dst=dst, pb=pb: e.activation(out=dst, in_=pb[:, :], func=AF.Copy)),
                             reads=[Rp], writes=[R_stage])
                    else:
                        S.op("dve", (lambda e, dst=dst, pb=pb: e.tensor_copy(out=dst, in_=pb[:, :])),
                             reads=[Rp], writes=[R_stage])
                ev = S.dma("sp", (lambda e, t0=t0: e.dma_start(out=y_out[s, t0:t0 + 128, :],
                                                               in_=self.fbuf[:, 2:4, :].rearrange("p a b -> p (a b)"))),
                           reads=[R_stage], semkey="ystore")
                evs.append(ev)
        return evs

    def prenorm(self, tt, gain, l, t0=None, n=TT):
        S = self.S
        if t0 is None:
            t0 = tt * TT
        tt = t0 // TT
        ts = slice(t0, t0 + n)
        Rx = self.R_x[tt]
        R_hn = self.R("hn")
        self.rms_stats([self.x_res[:, k, ts] for k in range(8)], [Rx], self.ones_bf, 8, n=n)
        R_rstd = self.R("rstd")
        for k in range(8):
            S.op("dve", (lambda e, k=k: e.scalar_tensor_tensor(out=self.hn[:, k, 0:n], in0=self.x_res[:, k, ts],
                                                               scalar=gain[:, l, k:k + 1], in1=self.rstd[:, 0:n],
                                                               op0=ALU.mult, op1=ALU.mult)),
                 reads=[Rx, R_rstd, self.R_c], writes=[R_hn])

    def rms_stats(self, srcs, src_res, ones, nt, psi=7, ones_res=None, n=TT, scale=1.0):
        S = self.S
        R_rstd = self.R("rstd")
        Rp = self.R_ps[psi]
        pb = self.psum[psi]
        for k in range(nt):
            Rs = self.R(f"sq{k % 2}")
            S.op("act", (lambda e, k=k: e.activation(out=self.sq[k % 2][:, 0:n], in_=srcs[k], func=AF.Square)),
                 reads=list(src_res), writes=[Rs])
            S.op("pe", (lambda e, k=k: e.matmul(pb[:, 0:n], lhsT=ones[:, :], rhs=self.sq[k % 2][:, 0:n],
                                                start=(k == 0), stop=(k == nt - 1))),
                 reads=[Rs, ones_res or self.R("ones")], writes=[Rp])
        S.op("act", lambda e: e.activation(out=self.rstd[:, 0:n], in_=pb[:, 0:n], func=AF.Sqrt, bias=self.eps_ap(), scale=1.0),
             reads=[Rp, self.R_c], writes=[R_rstd])
        S.op("dve", lambda e: e.reciprocal(out=self.rstd[:, 0:n], in_=self.rstd[:, 0:n]), reads=[R_rstd], writes=[R_rstd])

    def setup_extra(self):
        pass

    def eps_ap(self):
        return self.cst[:, 128:129]

    def epilogue(self, tt, gain, l, t0=None, n=TT):
        S = self.S
        if t0 is None:
            t0 = tt * TT
        tt = t0 // TT
        ts = slice(t0, t0 + n)
        R_f = self.R("fbuf")
        self.rms_stats([self.fbuf[:, k, 0:n] for k in range(8)], [R_f], self.ones_bf, 8, n=n)
        R_rstd = self.R("rstd")
        for k in range(8):
            Rt = self.R(f"tmpf{k % 2}")
            S.op("dve", (lambda e, k=k: e.scalar_tensor_tensor(out=self.tmpf[k % 2][:, 0:n], in0=self.fbuf[:, k, 0:n],
                                                               scalar=gain[:, l, k:k + 1], in1=self.rstd[:, 0:n],
                                                               op0=ALU.mult, op1=ALU.mult)),
                 reads=[R_f, R_rstd, self.R_c], writes=[Rt])
            S.op("pool", (lambda e, k=k: e.tensor_tensor(out=self.x_res[:, k, ts], in0=self.x_res[:, k, ts],
                                                         in1=self.tmpf[k % 2][:, 0:n], op=ALU.add)),
                 reads=[Rt], writes=[self.R_x[tt]])

    def load_w(self, src_ap, R_src):
        S = self.S
        i = self.slot_i
        self.slot_i = (self.slot_i + 1) % self.NSLOT
        q = "sp"
        S.dma(q, (lambda e, i=i, src_ap=src_ap: e.dma_start(out=self.wslot[i][:], in_=src_ap)),
              reads=[R_src], writes=[self.R_wslot[i]], semkey=f"wslot{i}")
        return self.wslot[i], self.R_wslot[i]

    def ffn_layer(self, l, sc_up, sc_dn, R_scr):
        S = self.S
        self.barrier()
        self.carve_reset()
        self.hid = self.carve([NHB, TT], BF16)
        self.gprev = self.carve([NHB, 2], F32)
        self.cv = [self.carve([TT], F32) for i in range(2)]
        self.ge = [self.carve([TT], F32) for i in range(2)]
        S.op("pool", lambda e: e.memset(self.gprev, 0.0), writes=[self.R("gprev")])
        for tt in range(NTT):
            self.prenorm(tt, self.lnw["ln_ffn_pre"], l)
            R_hn = self.R("hn")
            for hb in range(NHB):
                wg, Rwg = self.load_w(sc_up[l, 0, hb], R_scr)
                wu, Rwu = self.load_w(sc_up[l, 1, hb], R_scr)
                pg, Rpg = self.psum[(2 * hb) % 4], self.R_ps[(2 * hb) % 4]
                pu, Rpu = self.psum[(2 * hb) % 4 + 1], self.R_ps[(2 * hb) % 4 + 1]
                for k in range(8):
                    S.op("pe", (lambda e, k=k, wg=wg, pg=pg: e.matmul(pg[:, :], lhsT=wg[:, k, :], rhs=self.hn[:, k, :],
                                                                      start=(k == 0), stop=(k == 7))),
                         reads=[Rwg, R_hn], writes=[Rpg])
                for k in range(8):
                    S.op("pe", (lambda e, k=k, wu=wu, pu=pu: e.matmul(pu[:, :], lhsT=wu[:, k, :], rhs=self.hn[:, k, :],
                                                                      start=(k == 0), stop=(k == 7))),
                         reads=[Rwu, R_hn], writes=[Rpu])
                cv, Rcv = self.cv[hb % 2], self.R(f"cv{hb % 2}")
                ge, Rge = self.ge[hb % 2], self.R(f"ge{hb % 2}")
                w0 = self.fcw[:, l, 0, hb:hb + 1]
                w1 = self.fcw[:, l, 1, hb:hb + 1]
                w2 = self.fcw[:, l, 2, hb:hb + 1]
                bb = self.fcb[:, l, hb:hb + 1]
                Rgp = self.R("gprev")
                S.op("act", (lambda e, cv=cv, pg=pg, w2=w2, bb=bb: e.activation(out=cv[:], in_=pg[:, :], func=AF.Identity,
                                                                                bias=bb, scale=w2)),
                     reads=[Rpg, self.R_c], writes=[Rcv])
                S.op("dve", (lambda e, cv=cv, pg=pg, w1=w1: e.scalar_tensor_tensor(
                    out=cv[:, 1:TT], in0=pg[:, 0:TT - 1], scalar=w1, in1=cv[:, 1:TT], op0=ALU.mult, op1=ALU.add)),
                     reads=[Rpg, self.R_c], writes=[Rcv])
                S.op("dve", (lambda e, cv=cv, pg=pg, w0=w0: e.scalar_tensor_tensor(
                    out=cv[:, 2:TT], in0=pg[:, 0:TT - 2], scalar=w0, in1=cv[:, 2:TT], op0=ALU.mult, op1=ALU.add)),
                     reads=[Rpg, self.R_c], writes=[Rcv])
                S.op("dve", (lambda e, cv=cv, hb=hb, w0=w0: e.scalar_tensor_tensor(
                    out=cv[:, 0:2], in0=self.gprev[:, hb, 0:2], scalar=w0, in1=cv[:, 0:2], op0=ALU.mult, op1=ALU.add)),
                     reads=[Rgp, self.R_c], writes=[Rcv])
                S.op("dve", (lambda e, cv=cv, hb=hb, w1=w1: e.scalar_tensor_tensor(
                    out=cv[:, 0:1], in0=self.gprev[:, hb, 1:2], scalar=w1, in1=cv[:, 0:1], op0=ALU.mult, op1=ALU.add)),
                     reads=[Rgp, self.R_c], writes=[Rcv])
                S.op("act", (lambda e, pg=pg, hb=hb: e.activation(out=self.gprev[:, hb, :], in_=pg[:, TT - 2:TT], func=AF.Copy)),
                     reads=[Rpg, Rcv], writes=[Rgp])
                S.op("act", (lambda e, cv=cv, ge=ge: e.activation(out=ge[:], in_=cv[:], func=AF.Gelu_apprx_tanh)),
                     reads=[Rcv], writes=[Rge])
                S.op("dve", (lambda e, ge=ge, pu=pu, hb=hb: e.tensor_tensor(out=self.hid[:, hb, :], in0=ge[:], in1=pu[:, :],
                                                                            op=ALU.mult)),
                     reads=[Rge, Rpu], writes=[self.R_hid[hb]])
            R_f = self.R("fbuf")
            for hb in range(NHB):
                wd, Rwd = self.load_w(sc_dn[l, hb], R_scr)
                for ob in range(8):
                    S.op("pe", (lambda e, hb=hb, ob=ob, wd=wd: e.matmul(
                        self.psum[ob][:, :], lhsT=wd[:, ob, :], rhs=self.hid[:, hb, :],
                        start=(hb == 0), stop=(hb == NHB - 1))),
                         reads=[Rwd, self.R_hid[hb]], writes=[self.R_ps[ob]])
            for ob in range(8):
                if ob % 2 == 0:
                    S.op("act", (lambda e, ob=ob: e.activation(out=self.fbuf[:, ob, :], in_=self.psum[ob][:, :],
                                                               func=AF.Copy)),
                         reads=[self.R_ps[ob]], writes=[R_f])
                else:
                    S.op("dve", (lambda e, ob=ob: e.tensor_copy(out=self.fbuf[:, ob, :], in_=self.psum[ob][:, :])),
                         reads=[self.R_ps[ob]], writes=[R_f])
            self.epilogue(tt, self.lnw["ln_ffn_post"], l)


    def rr3(self, eng, out, x, tmp, R, W):
        MAGIC = 12582912.0
        self.I(eng, "tensor_scalar", R=R, W=W, out=tmp, in0=x, scalar1=float(1.0 / (2 * np.pi)), scalar2=MAGIC,
               op0=ALU.mult, op1=ALU.add)
        self.I(eng, "tensor_scalar", R=W, W=W, out=tmp, in0=tmp, scalar1=-MAGIC, scalar2=float(-2 * np.pi),
               op0=ALU.add, op1=ALU.mult)
        self.I(eng, "tensor_tensor", R=list(R) + list(W), W=W, out=out, in0=x, in1=tmp, op=ALU.add)

    def mix_even(self, l):
        S, I, c = self.S, self.I, self.cst
        e = l // 2
        inp = self.inp
        R_scr = self.R("scratch_w")
        self.barrier()
        self.carve_reset()
        CV = self.carve
        ident = c[:, 0:128]
        Rc = self.R_c
        lre, lim, ldt = CV([16]), CV([16]), CV([16])
        t_a, t_b, t_c, t_d = CV([16]), CV([16]), CV([16]), CV([16])
        rmag, thr, zr, zi = CV([16]), CV([16]), CV([16]), CV([16])
        phi0, phit = CV([16]), CV([16])
        dsk, bglu, balp, gnorm = CV([4]), CV([4]), CV([4]), CV([4])
        bul = CV([16, 2, 128], BF16)
        cl = CV([16, 3, 128], BF16)
        wup = CV([256], BF16)
        wglu = CV([4, 512], BF16)
        walr = CV([8, 16], BF16)
        ycat = CV([8, TT], BF16)
        st = CV([4, 128])
        st_bf = CV([4, 128], BF16)
        zprev = CV([16, 2])
        mark = self.aoff
        Bre, Bim = CV([16, 16]), CV([16, 16])
        bbre, bbim, bbt = CV([16, 16]), CV([16, 16]), CV([16, 16])
        Cn = [CV([4, 64]), CV([4, 64])]
        Tin = [[CV([128]) for ri in range(2)] for q in range(4)]
        Tin2 = [CV([128]) for ri in range(2)]
        self.aoff = mark
        u_bf = CV([4, TT], BF16)
        cosT, sinT, tba, tbk = CV([TT]), CV([TT]), CV([TT]), CV([TT])
        zin = [CV([TT]), CV([TT])]
        zt = CV([TT])
        zz = [CV([TT]), CV([TT])]
        prods = [CV([TT], BF16) for i in range(4)]
        ysb = CV([TT])
        yg = CV([4, TT], BF16)
        end1 = self.aoff
        self.aoff = mark
        alr_bf = CV([TT], BF16)
        lsb = CV([TT])
        Scum = CV([TT])
        eq, ek = CV([TT]), CV([TT])
        eglast = CV([4, 8])
        q_dec, k_dec = CV([4, TT], BF16), CV([4, TT], BF16)
        k_upd = CV([4, TT])
        v_tm = CV([8, 512], BF16)
        sT = CV([256], BF16)
        kupd_tm = CV([256], BF16)
        o_sb = CV([4, TT])
        self.aoff = max(end1, self.aoff)
        silr = ycat[:, 4:8, :]
        ones128 = CV([128], BF16)
        negb = CV([4])
        Rn = self.R
        R_par = Rn("ev_par")

        def pd(out, in_, q="sp"):
            S.dma(q, (lambda e_, out=out, in_=in_: e_.dma_start(out=out, in_=in_, allow_slow_non_contiguous=True)),
                  semkey="evpar")
        pd(lre, inp["s5_lambda_re"][e].rearrange("(pr g2) p -> (g2 p) pr", g2=2))
        pd(lim, inp["s5_lambda_im"][e].rearrange("(pr g2) p -> (g2 p) pr", g2=2))
        ldt_src = inp["s5_log_dt"][e].rearrange("(pr g2) -> g2 pr", g2=2)
        for g2 in range(2):
            pd(ldt[64 * g2:64 * g2 + 64, :], ldt_src[g2:g2 + 1, :].partition_broadcast(64))
        pd(Bre, inp["s5_b_re"][e].rearrange("(pr g2) p j -> (g2 p) pr j", g2=2))
        pd(Bim, inp["s5_b_im"][e].rearrange("(pr g2) p j -> (g2 p) pr j", g2=2))
        pd(Cn[0], inp["s5_c_re"][e].rearrange("(ft g8) i p -> (g8 i) ft p", g8=8))
        pd(Cn[1], inp["s5_c_im"][e].rearrange("(ft g8) i p -> (g8 i) ft p", g8=8))
        pd(dsk, inp["s5_d"][e].rearrange("(ft g8) i -> (g8 i) ft", g8=8))
        pd(bglu, inp["s5_b_glu"][e].rearrange("(t p) -> p t", p=128))
        pd(balp[0:64, :], inp["gla_b_alpha"][e].rearrange("(h p) -> p h", p=64))
        pd(gnorm, inp["gla_norm"][e].rearrange("(t p) -> p t", p=128))
        pd(wup[0:16, :], inp["gla_w_alpha_up"][e], q="pool")
        pd(wglu, inp["s5_w_glu"][e].rearrange("(kt p) c -> p kt c", p=128), q="pool")
        pd(walr, inp["ev_w_in"][e][:, 2048:2064].rearrange("(kt p) c -> p kt c", p=128), q="pool")
        R_par.w = (("d", "evpar"), 16 * S.dcnt[("d", "evpar")])
        R_par.r = []

        Rt = Rn("ev_tiny")
        P = [R_par, Rc]
        I("act", "activation", R=P, W=[Rt], out=t_a, in_=ldt, func=AF.Exp)
        I("dve", "tensor_tensor", R=P + [Rt], W=[Rt], out=t_b, in0=lre, in1=t_a, op=ALU.mult)
        I("act", "activation", R=[Rt], W=[Rt], out=rmag, in_=t_b, func=AF.Exp)
        I("dve", "tensor_tensor", R=P + [Rt], W=[Rt], out=t_b, in0=lim, in1=t_a, op=ALU.mult)
        self.rr3("dve", thr, t_b, t_c, [Rt], [Rt])
        I("act", "activation", R=[Rt], W=[Rt], out=t_a, in_=thr, func=AF.Sin)
        I("act", "activation", R=[Rt], W=[Rt], out=t_b, in_=thr, func=AF.Abs)
        I("act", "activation", R=[Rt, Rc], W=[Rt], out=t_b, in_=t_b, func=AF.Sin, scale=-1.0, bias=c[:, 129:130])
        I("dve", "tensor_tensor", R=[Rt], W=[Rt], out=t_a, in0=t_a, in1=rmag, op=ALU.mult)
        I("dve", "tensor_tensor", R=[Rt], W=[Rt], out=t_b, in0=t_b, in1=rmag, op=ALU.mult)
        I("dve", "tensor_scalar", R=[Rt], W=[Rt], out=t_b, in0=t_b, scalar1=-1.0, scalar2=None, op0=ALU.add)
        I("dve", "tensor_tensor", R=P + [Rt], W=[Rt], out=t_c, in0=lre, in1=lre, op=ALU.mult)
        I("dve", "tensor_tensor", R=P + [Rt], W=[Rt], out=t_d, in0=lim, in1=lim, op=ALU.mult)
        I("dve", "tensor_tensor", R=[Rt], W=[Rt], out=t_c, in0=t_c, in1=t_d, op=ALU.add)
        I("dve", "reciprocal", R=[Rt], W=[Rt], out=t_c, in_=t_c)
        I("dve", "tensor_tensor", R=P + [Rt], W=[Rt], out=zr, in0=t_b, in1=lre, op=ALU.mult)
        I("dve", "tensor_tensor", R=P + [Rt], W=[Rt], out=t_d, in0=t_a, in1=lim, op=ALU.mult)
        I("dve", "tensor_tensor", R=[Rt], W=[Rt], out=zr, in0=zr, in1=t_d, op=ALU.add)
        I("dve", "tensor_tensor", R=[Rt], W=[Rt], out=zr, in0=zr, in1=t_c, op=ALU.mult)
        I("dve", "tensor_tensor", R=P + [Rt], W=[Rt], out=zi, in0=t_a, in1=lre, op=ALU.mult)
        I("dve", "tensor_tensor", R=P + [Rt], W=[Rt], out=t_d, in0=t_b, in1=lim, op=ALU.mult)
        I("dve", "tensor_tensor", R=[Rt], W=[Rt], out=zi, in0=zi, in1=t_d, op=ALU.subtract)
        I("dve", "tensor_tensor", R=[Rt], W=[Rt], out=zi, in0=zi, in1=t_c, op=ALU.mult)
        zr_b, zi_b = self.bc(zr, 16), self.bc(zi, 16)
        I("dve", "tensor_tensor", R=P + [Rt], W=[Rt], out=bbre, in0=Bre, in1=zr_b, op=ALU.mult)
        I("dve", "tensor_tensor", R=P + [Rt], W=[Rt], out=bbt, in0=Bim, in1=zi_b, op=ALU.mult)
        I("dve", "tensor_tensor", R=[Rt], W=[Rt], out=bbre, in0=bbre, in1=bbt, op=ALU.subtract)
        I("dve", "tensor_tensor", R=P + [Rt], W=[Rt], out=bbim, in0=Bim, in1=zr_b, op=ALU.mult)
        I("dve", "tensor_tensor", R=P + [Rt], W=[Rt], out=bbt, in0=Bre, in1=zi_b, op=ALU.mult)
        I("dve", "tensor_tensor", R=[Rt], W=[Rt], out=bbim, in0=bbim, in1=bbt, op=ALU.add)
        I("dve", "tensor_scalar", R=P, W=[Rt], out=negb[0:64, :], in0=balp[0:64, :], scalar1=-1.0, scalar2=None, op0=ALU.mult)
        I("dve", "memset", W=[Rn("ones128")], ap=ones128, constant=1.0 / 128)
        for q in range(4):
            for ri in range(2):
                I("pool", "memset", W=[Rn(f"Tin{q}{ri}")], ap=Tin[q][ri], constant=0.0)
        for pr in range(16):
            q = pr % 4
            for ri, bb in enumerate((bbre, bbim)):
                RT = Rn(f"Tin{q}{ri}")
                I("dve", "tensor_copy", R=[Rt], W=[RT], out=Tin[q][ri][0:64, 32 * q:32 * q + 16], in_=bb[0:64, pr, :])
                I("dve", "tensor_copy", R=[Rt], W=[RT], out=Tin[q][ri][64:128, 32 * q + 16:32 * q + 32], in_=bb[64:128, pr, :])
                pb, Rp = self.psum[ri], self.R_ps[ri]
                I("pe", "transpose", R=[RT, Rc], W=[Rp], out=pb[:, 0:128], in_=Tin[q][ri], identity=ident)
                I("act", "activation", R=[Rp], W=[Rn("bul")], out=bul[:, pr, ri, :], in_=pb[:, 0:128], func=AF.Copy)
        for pr in range(16):
            ft, q = pr // 4, pr % 4
            for ri in range(2):
                RT = Rn(f"Tin2{ri}")
                I("dve", "tensor_scalar", R=P, W=[RT], out=Tin2[ri][:, 0:64], in0=Cn[ri][:, ft, :],
                  scalar1=c[:, 136 + 2 * q:137 + 2 * q], scalar2=None, op0=ALU.mult)
                I("dve", "tensor_scalar", R=P, W=[RT], out=Tin2[ri][:, 64:128], in0=Cn[ri][:, ft, :],
                  scalar1=c[:, 137 + 2 * q:138 + 2 * q], scalar2=None, op0=ALU.mult)
                pb, Rp = self.psum[2 + ri], self.R_ps[2 + ri]
                I("pe", "transpose", R=[RT, Rc], W=[Rp], out=pb[:, 0:128], in_=Tin2[ri], identity=ident)
                if ri == 0:
                    I("act", "activation", R=[Rp], W=[Rn("cl")], out=cl[:, pr, 0, :], in_=pb[:, 0:128], func=AF.Copy)
                    I("act", "activation", R=[Rp], W=[Rn("cl")], out=cl[:, pr, 1, :], in_=pb[:, 0:128], func=AF.Copy, scale=-1.0)
                else:
                    I("act", "activation", R=[Rp], W=[Rn("cl")], out=cl[:, pr, 2, :], in_=pb[:, 0:128], func=AF.Copy, scale=-1.0)
        I("pool", "memset", W=[Rn("zprev")], ap=zprev, constant=0.0)
        I("pool", "memset", W=[Rn("st")], ap=st, constant=0.0)
        I("pool", "memset", W=[Rn("st_bf")], ap=st_bf, constant=0.0)

        sc_ein, sc_eout = self.sc_ein, self.sc_eout
        R_hn = Rn("hn")
        maskT = c[0:64, 256:320]
        iota = c[:, 512:1024]
        cmask = c[:, 1024:1536]

        def proj(blk, psi):
            w, Rw = self.load_w(sc_ein[e, blk], R_scr)
            for k in range(8):
                I("pe", "matmul", R=[Rw, R_hn], W=[self.R_ps[psi]], out=self.psum[psi][:, :], lhsT=w[:, k, :],
                  rhs=self.hn[:, k, :], start=(k == 0), stop=(k == 7))
            return self.psum[psi], self.R_ps[psi]

        import os
        stage = int(os.environ.get('DBG_STAGE', '9'))
        self.barrier()
        for tt in range(NTT if stage > 0 else 0):
            t0 = tt * TT
            self.prenorm(tt, self.lnw["ln_mix_pre"], l)
            for ft in range(4):
                pb, Rp = proj(ft, ft % 2)
                I("act", "activation", R=[Rp], W=[Rn("u_bf")], out=u_bf[:, ft, :], in_=pb[:, :], func=AF.Copy)
            I("dve", "tensor_scalar", R=[Rt], W=[Rn("phi")], out=phit, in0=thr, scalar1=float(t0), scalar2=None, op0=ALU.mult)
            self.rr3("dve", phi0, phit, t_d, [Rn("phi")], [Rn("phi")])
            for ft in range(4):
                for q in range(4):
                    pr = 4 * ft + q
                    pre, Rpre = self.psum[2], self.R_ps[2]
                    pim, Rpim = self.psum[3], self.R_ps[3]
                    I("pe", "matmul", R=[Rn("bul"), Rn("u_bf")], W=[Rpre], out=pre[:, :], lhsT=bul[:, pr, 0, :], rhs=u_bf[:, ft, :],
                      start=True, stop=True)
                    I("pe", "matmul", R=[Rn("bul"), Rn("u_bf")], W=[Rpim], out=pim[:, :], lhsT=bul[:, pr, 1, :], rhs=u_bf[:, ft, :],
                      start=True, stop=True)
                    Rtab = Rn("tab")
                    I("pool", "tensor_scalar", R=[Rt, Rn("phi"), Rc], W=[Rn("tba")], out=tba, in0=iota, scalar1=thr[:, pr:pr + 1],
                      scalar2=phi0[:, pr:pr + 1], op0=ALU.mult, op1=ALU.add)
                    self.rr3("pool", tba, tba, tbk, [Rn("tba")], [Rn("tba"), Rn("tbk")])
                    I("act", "activation", R=[Rn("tba")], W=[Rn("sinT")], out=sinT, in_=tba, func=AF.Sin)
                    I("act", "activation", R=[Rn("tba")], W=[Rn("tbk")], out=tbk, in_=tba, func=AF.Abs)
                    I("act", "activation", R=[Rn("tbk"), Rc], W=[Rn("cosT")], out=cosT, in_=tbk, func=AF.Sin, scale=-1.0,
                      bias=c[:, 129:130])
                    I("dve", "tensor_tensor", R=[Rn("cosT"), Rpre], W=[Rn("zin0")], out=zin[0], in0=cosT, in1=pre[:, :], op=ALU.mult)
                    I("dve", "tensor_tensor", R=[Rn("sinT"), Rpim], W=[Rn("zt")], out=zt, in0=sinT, in1=pim[:, :], op=ALU.mult)
                    I("dve", "tensor_tensor", R=[Rn("zt")], W=[Rn("zin0")], out=zin[0], in0=zin[0], in1=zt, op=ALU.add)
                    I("dve", "tensor_tensor", R=[Rn("cosT"), Rpim], W=[Rn("zin1")], out=zin[1], in0=cosT, in1=pim[:, :], op=ALU.mult)
                    I("dve", "tensor_tensor", R=[Rn("sinT"), Rpre], W=[Rn("zt")], out=zt, in0=sinT, in1=pre[:, :], op=ALU.mult)
                    I("dve", "tensor_tensor", R=[Rn("zt")], W=[Rn("zin1")], out=zin[1], in0=zin[1], in1=zt, op=ALU.subtract)
                    for ri in range(2):
                        I("dve", "tensor_tensor_scan", R=[Rn(f"zin{ri}"), Rt, Rn("zprev")], W=[Rn(f"zz{ri}")], out=zz[ri],
                          data0=rmag[:, pr:pr + 1].to_broadcast([128, TT]),
                          data1=zin[ri], initial=zprev[:, pr, ri:ri + 1], op0=ALU.mult, op1=ALU.add)
                        I("act", "activation", R=[Rn(f"zz{ri}")], W=[Rn("zprev")], out=zprev[:, pr, ri:ri + 1],
                          in_=zz[ri][:, TT - 1:TT], func=AF.Copy)
                    I("pool", "tensor_tensor", R=[Rn("cosT"), Rn("zz0")], W=[Rn("pr0")], out=prods[0], in0=cosT, in1=zz[0], op=ALU.mult)
                    I("pool", "tensor_tensor", R=[Rn("sinT"), Rn("zz1")], W=[Rn("pr1")], out=prods[1], in0=sinT, in1=zz[1], op=ALU.mult)
                    I("pool", "tensor_tensor", R=[Rn("sinT"), Rn("zz0")], W=[Rn("pr2")], out=prods[2], in0=sinT, in1=zz[0], op=ALU.mult)
                    I("pool", "tensor_tensor", R=[Rn("cosT"), Rn("zz1")], W=[Rn("pr3")], out=prods[3], in0=cosT, in1=zz[1], op=ALU.mult)
                    py, Rpy = self.psum[4], self.R_ps[4]
                    for i, var in enumerate((0, 1, 2, 2)):
                        I("pe", "matmul", R=[Rn("cl"), Rn(f"pr{i}")], W=[Rpy], out=py[:, :], lhsT=cl[:, pr, var, :], rhs=prods[i],
                          start=(q == 0 and i == 0), stop=(q == 3 and i == 3))
                py, Rpy = self.psum[4], self.R_ps[4]
                I("dve", "scalar_tensor_tensor", R=[Rn("u_bf"), P[0], Rpy], W=[Rn("ysb")], out=ysb, in0=u_bf[:, ft, :],
                  scalar=dsk[:, ft:ft + 1], in1=py[:, :], op0=ALU.mult, op1=ALU.add)
                I("act", "activation", R=[Rn("ysb")], W=[Rn("yg")], out=yg[:, ft, :], in_=ysb, func=AF.Gelu_apprx_tanh)
            for ct in range(4):
                pb, Rp = self.psum[5], self.R_ps[5]
                for k in range(4):
                    I("pe", "matmul", R=[R_par, Rn("yg")], W=[Rp], out=pb[:, :], lhsT=wglu[:, k, ct * 128:(ct + 1) * 128],
                      rhs=yg[:, k, :], start=(k == 0), stop=(k == 3))
                I("act", "activation", R=[Rp, R_par], W=[Rn("ysb")], out=ysb, in_=pb[:, :], func=AF.Sigmoid, bias=bglu[:, ct:ct + 1])
                I("pool", "tensor_tensor", R=[Rn("ysb"), Rn("yg")], W=[Rn("ycat")], out=ycat[:, ct, :], in0=yg[:, ct, :], in1=ysb,
                  op=ALU.mult)
            if stage < 2:
                continue
            self.barrier()
            pb, Rp = self.psum[0], self.R_ps[0]
            for k in range(8):
                I("pe", "matmul", R=[R_par, R_hn], W=[Rp], out=pb[0:16, :], lhsT=walr[:, k, :], rhs=self.hn[:, k, :],
                  start=(k == 0), stop=(k == 7))
            I("act", "activation", R=[Rp], W=[Rn("alr")], out=alr_bf[0:16, :], in_=pb[0:16, :], func=AF.Copy)
            for t2 in range(2):
                wq, Rwq = self.load_w(sc_ein[e, 4 + t2], R_scr)
                wk, Rwk = self.load_w(sc_ein[e, 6 + t2], R_scr)
                for hh in range(2):
                    h = 2 * t2 + hh
                    pb, Rp = self.psum[1], self.R_ps[1]
                    I("pe", "matmul", R=[R_par, Rn("alr")], W=[Rp], out=pb[0:64, :], lhsT=wup[0:16, h * 64:(h + 1) * 64],
                      rhs=alr_bf[0:16, :], start=True, stop=True)
                    I("act", "activation", R=[Rp, Rt], W=[Rn("lsb")], out=lsb[0:64, :], in_=pb[0:64, :], func=AF.Exp, scale=-1.0,
                      bias=negb[0:64, h:h + 1])
                    I("act", "activation", R=[Rn("lsb"), Rc], W=[Rn("lsb")], out=lsb[0:64, :], in_=lsb[0:64, :], func=AF.Ln,
                      bias=c[0:64, 130:131])
                    I("dve", "tensor_tensor_scan", R=[Rn("lsb"), Rc], W=[Rn("Scum")], out=Scum[0:64, :], data0=cmask[0:64, :],
                      data1=lsb[0:64, :], initial=0.0, op0=ALU.mult, op1=ALU.add)
                    I("act", "activation", R=[Rn("Scum")], W=[Rn("eq")], out=eq[0:64, :], in_=Scum[0:64, :], func=AF.Exp,
                      scale=-1.0 / 16)
                    I("act", "activation", R=[Rn("Scum")], W=[Rn("ek")], out=ek[0:64, :], in_=Scum[0:64, :], func=AF.Exp,
                      scale=1.0 / 16)
                    I("act", "activation", R=[Rn("Scum")], W=[Rn("eglast")], out=eglast[0:64, h, :],
                      in_=Scum[0:64, :].rearrange("p (c t) -> p c t", t=64)[:, :, 63], func=AF.Exp, scale=-1.0 / 16)
                    pq, Rpq = self.psum[2], self.R_ps[2]
                    for k in range(8):
                        I("pe", "matmul", R=[Rwq, R_hn], W=[Rpq], out=pq[0:64, :], lhsT=wq[:, k, hh * 64:(hh + 1) * 64],
                          rhs=self.hn[:, k, :], start=(k == 0), stop=(k == 7))
                    I("dve", "scalar_tensor_tensor", R=[Rpq, Rn("eq")], W=[Rn("q_dec")], out=q_dec[0:64, h, :], in0=pq[0:64, :],
                      scalar=0.125, in1=eq[0:64, :], op0=ALU.mult, op1=ALU.mult)
                    pk, Rpk = self.psum[3], self.R_ps[3]
                    for k in range(8):
                        I("pe", "matmul", R=[Rwk, R_hn], W=[Rpk], out=pk[0:64, :], lhsT=wk[:, k, hh * 64:(hh + 1) * 64],
                          rhs=self.hn[:, k, :], start=(k == 0), stop=(k == 7))
                    I("dve", "tensor_tensor", R=[Rpk, Rn("ek")], W=[Rn("k_dec")], out=k_dec[0:64, h, :], in0=pk[0:64, :],
                      in1=ek[0:64, :], op=ALU.mult)
                    I("pool", "tensor_tensor", R=[Rn("k_dec"), Rn("eglast")], W=[Rn("k_upd")],
                      out=k_upd[0:64, h, :].rearrange("p (c t) -> p c t", t=64),
                      in0=k_dec[0:64, h, :].rearrange("p (c t) -> p c t", t=64), in1=self.bc(eglast[0:64, h, :], 64), op=ALU.mult)
            for h in range(4):
                pb, Rp = proj(12 + h, h % 2)
                I("act", "activation", R=[Rp], W=[Rn("silr")], out=silr[:, h, :], in_=pb[:, :], func=AF.Silu)
            wv = [self.load_w(sc_ein[e, 8 + b], R_scr) for b in range(4)]
            for ch in range(8):
                pb, Rp = self.psum[ch % 2], self.R_ps[ch % 2]
                for b in range(4):
                    for k in range(8):
                        I("pe", "matmul", R=[wv[b][1], R_hn], W=[Rp], out=pb[0:64, b * 128:(b + 1) * 128],
                          lhsT=self.hn[:, k, ch * 64:(ch + 1) * 64], rhs=wv[b][0][:, k, :], start=(k == 0), stop=(k == 7))
                I("act", "activation", R=[Rp], W=[Rn("v_tm")], out=v_tm[0:64, ch, :], in_=pb[0:64, :], func=AF.Copy)
            if stage < 3:
                continue
            for ch in range(8):
                cs = slice(ch * 64, (ch + 1) * 64)
                ps_s, Rs_ = self.psum[2], self.R_ps[2]
                for h in range(4):
                    I("pe", "matmul", R=[Rn("k_dec"), Rn("q_dec")], W=[Rs_], out=ps_s[0:64, h * 64:(h + 1) * 64],
                      lhsT=k_dec[0:64, h, cs], rhs=q_dec[0:64, h, cs], start=True, stop=True)
                I("dve", "tensor_tensor", R=[Rs_, Rc], W=[Rn("sT")], out=sT[0:64, :].rearrange("p (h i) -> p h i", h=4),
                  in0=ps_s[0:64, 0:256].rearrange("p (h i) -> p h i", h=4), in1=self.bc(maskT, 4, pos=0), op=ALU.mult)
                ps_t, Rt_ = self.psum[3], self.R_ps[3]
                for h in range(4):
                    I("pe", "transpose", R=[Rn("k_upd"), Rc], W=[Rt_], out=ps_t[0:64, h * 64:(h + 1) * 64],
                      in_=k_upd[0:64, h, cs], identity=c[0:64, 0:64])
                I("act", "activation", R=[Rt_], W=[Rn("kupd_tm")], out=kupd_tm[0:64, :], in_=ps_t[0:64, 0:256], func=AF.Copy)
                ps_o, Ro_ = self.psum[5], self.R_ps[5]
                for h in range(4):
                    I("pe", "matmul", R=[Rn("v_tm"), Rn("sT")], W=[Ro_], out=ps_o[:, h * 64:(h + 1) * 64],
                      lhsT=v_tm[0:64, ch, h * 128:(h + 1) * 128], rhs=sT[0:64, h * 64:(h + 1) * 64], start=True, stop=False)
                    I("pe", "matmul", R=[Rn("st_bf"), Rn("q_dec")], W=[Ro_], out=ps_o[:, h * 64:(h + 1) * 64],
                      lhsT=st_bf[0:64, h, :], rhs=q_dec[0:64, h, cs], start=False, stop=True)
                I("act", "activation", R=[Ro_], W=[Rn("o_sb")], out=o_sb[:, :, cs],
                  in_=ps_o[:, 0:256].rearrange("p (h i) -> p h i", h=4), func=AF.Copy)
                ps_d, Rd_ = self.psum[6], self.R_ps[6]
                for h in range(4):
                    I("pe", "matmul", R=[Rn("kupd_tm"), Rn("v_tm")], W=[Rd_], out=ps_d[0:64, h * 128:(h + 1) * 128],
                      lhsT=kupd_tm[0:64, h * 64:(h + 1) * 64], rhs=v_tm[0:64, ch, h * 128:(h + 1) * 128], start=True, stop=True)
                for h in range(4):
                    I("dve", "scalar_tensor_tensor", R=[Rd_, Rn("eglast")], W=[Rn("st")], out=st[0:64, h, :],
                      in0=st[0:64, h, :], scalar=eglast[0:64, h, ch:ch + 1], in1=ps_d[0:64, h * 128:(h + 1) * 128],
                      op0=ALU.mult, op1=ALU.add)
                I("act", "activation", R=[Rn("st")], W=[Rn("st_bf")], out=st_bf[0:64, :, :], in_=st[0:64, :, :], func=AF.Copy)
            if stage < 4:
                continue
            for h in range(4):
                self.rms_stats([o_sb[:, h, :]], [Rn("o_sb")], ones128, 1, ones_res=Rn("ones128"))
                Rtm = Rn("tmpf0")
                I("dve", "scalar_tensor_tensor", R=[Rn("o_sb"), Rn("rstd"), R_par], W=[Rtm], out=self.tmpf[0][:], in0=o_sb[:, h, :],
                  scalar=gnorm[:, h:h + 1], in1=self.rstd[:], op0=ALU.mult, op1=ALU.mult)
                I("pool", "tensor_tensor", R=[Rtm, Rn("silr")], W=[Rn("silr")], out=ycat[:, 4 + h, :], in0=self.tmpf[0][:],
                  in1=silr[:, h, :], op=ALU.mult)
            R_f = Rn("fbuf")
            for ob in range(8):
                w, Rw = self.load_w(sc_eout[e, ob], R_scr)
                pb, Rp = self.psum[ob % 2], self.R_ps[ob % 2]
                for k in range(8):
                    I("pe", "matmul", R=[Rw, Rn("ycat"), Rn("silr")], W=[Rp], out=pb[:, :], lhsT=w[:, k, :], rhs=ycat[:, k, :],
                      start=(k == 0), stop=(k == 7))
                I("act", "activation", R=[Rp], W=[R_f], out=self.fbuf[:, ob, :], in_=pb[:, :], func=AF.Copy)
            self.epilogue(tt, self.lnw["ln_mix_post"], l)
            self.barrier()


    def prologue_odd(self, used_layers):
        S, inp = self.S, self.inp
        self.sc_oin = self.dscratch("sc_oin", [2, 32, 128, 8, 128])
        self.sc_oout = self.dscratch("sc_oout", [2, 8, 2, 128, 8, 128])
        for o in sorted(set(l // 2 for l in used_layers if l % 2 == 1)):
            for fo in range(32):
                src = inp["od_w_in"][o, :, fo * 128:(fo + 1) * 128].rearrange("(kt p) c -> p kt c", p=128)
                S.dma("pool", (lambda e, dst=self.sc_oin[o, fo], src=src: e.dma_start(out=dst, in_=src)), semkey="prolog")
            for ob in range(8):
                for hf in range(2):
                    src = inp["od_w_out"][o, hf * 1024:(hf + 1) * 1024, ob * 128:(ob + 1) * 128].rearrange(
                        "(kt p) c -> p kt c", p=128)
                    S.dma("pool", (lambda e, dst=self.sc_oout[o, ob, hf], src=src: e.dma_start(out=dst, in_=src)),
                          semkey="prolog")

    def mix_odd(self, l):
        S, I, c = self.S, self.I, self.cst
        o = l // 2
        inp = self.inp
        Rn = self.R
        Rc = self.R_c
        R_scr = Rn("scratch_w")
        self.barrier()
        self.carve_reset()
        CV = self.carve
        TM = 64
        DHS = float(512 ** -0.5)
        ident = c[:, 0:128]
        tri = c[0:64, 320:384]
        maskT = c[0:64, 256:320]
        BDM = c[:, 384:512]
        Cst = CV([16, 512])
        Cbf = CV([4, 512], BF16)
        nst, nbf = CV([16]), CV([16], BF16)
        mrow = CV([1])
        hist = CV([16, 3])
        Wq, Wk, Wv = CV([16, 128], BF16), CV([16, 128], BF16), CV([16, 128], BF16)
        wgt = CV([3, 16, 8], BF16)
        cw, cb, ng, sk = CV([16, 4]), CV([16]), CV([16]), CV([16])
        bg_bc = CV([8])
        onesb = CV([8], BF16)
        wraw = CV([3, 16, 4])
        xmT, xcT, qT, kT, vT, ogT = [CV([16, TM], BF16) for _ in range(6)]
        cvb = CV([TM])
        k_tm, v_tm = CV([2048], BF16), CV([2048], BF16)
        g_tm, lpos, a_tm = CV([8]), CV([4]), CV([4])
        a_row, F_row, M_row, wi_row, em_row, wu_row = [CV([64]) for _ in range(6)]
        negM, diagd, dec_bc, pk, sclk = CV([1]), CV([4]), CV([4]), CV([12]), CV([4])
        E, sTf, sT = CV([256]), CV([256]), CV([256], BF16)
        dint, den, nden, rden, wr, ss, rs = [CV([4]) for _ in range(7)]
        t1, hh = CV([512]), CV([512])
        u1, u2 = CV([4, 64]), CV([4, 64])
        R_par = Rn("od_par")

        def pd(out, in_, q="sp"):
            S.dma(q, (lambda e_, out=out, in_=in_: e_.dma_start(out=out, in_=in_, allow_slow_non_contiguous=True)),
                  semkey="odpar")
        for si, nm in enumerate(("ml_w_q", "ml_w_k", "ml_w_v")):
            pd(wraw[:, si, :, :], inp[nm][o].rearrange("(ft nl) c d -> (nl c) ft d", nl=32))
        for si in range(3):
            pd(wgt[:, si, :, :], inp["ml_w_gate"][o, si].rearrange("(ft p) g -> p ft g", p=128), q="pool")
        for k in range(4):
            pd(cw[:, :, k], inp["ml_conv_w"][o, k].rearrange("(ft p) -> p ft", p=128))
        pd(cb, inp["ml_conv_b"][o].rearrange("(ft p) -> p ft", p=128))
        pd(ng, inp["ml_norm"][o].rearrange("(ft p) -> p ft", p=128))
        pd(sk, inp["ml_skip"][o].rearrange("(ft p) -> p ft", p=128))
        pd(bg_bc[0:64, :], inp["ml_b_gate"][o:o + 1, :].partition_broadcast(64))
        R_par.w = (("d", "odpar"), 16 * S.dcnt[("d", "odpar")])
        R_par.r = []
        P = [R_par, Rc]
        for si, W in enumerate((Wq, Wk, Wv)):
            for ft in range(16):
                I("dve" if ft % 2 == 0 else "pool", "tensor_tensor", R=P, W=[Rn("Wbd")],
                  out=W[:, ft, :].rearrange("p (n d) -> p n d", d=4), in0=BDM.rearrange("p (n d) -> p n d", d=4),
                  in1=self.bc(wraw[:, si, ft, :], 32, pos=0), op=ALU.mult)
        I("pool", "memset", W=[Rn("Cst")], ap=Cst, constant=0.0)
        I("pool", "memset", W=[Rn("nst")], ap=nst, constant=0.0)
        I("pool", "memset", W=[Rn("nbf")], ap=nbf, constant=0.0)
        I("pool", "memset", W=[Rn("mrow")], ap=mrow, constant=0.0)
        I("pool", "memset", W=[Rn("hist")], ap=hist, constant=0.0)
        I("pool", "memset", W=[Rn("onesb")], ap=onesb, constant=1.0)
        self.barrier()
        R_hn = Rn("hn")
        sc_oin, sc_oout = self.sc_oin, self.sc_oout
        import os
        stage = int(os.environ.get('DBG_STAGE', '9'))
        nchunks = int(os.environ.get('DBG_NCH', str(L // TM)))

        for ch in range(nchunks):
            t0 = ch * TM
            self.prenorm(None, self.lnw["ln_mix_pre"], l, t0=t0, n=TM)
            for fo in range(32):
                w, Rw = self.load_w(sc_oin[o, fo], R_scr)
                pb, Rp = self.psum[fo % 2], self.R_ps[fo % 2]
                for k in range(8):
                    I("pe", "matmul", R=[Rw, R_hn], W=[Rp], out=pb[:, 0:TM], lhsT=w[:, k, :], rhs=self.hn[:, k, 0:TM],
                      start=(k == 0), stop=(k == 7))
                if fo < 16:
                    ft = fo
                    I("act", "activation", R=[Rp], W=[Rn("xmT")], out=xmT[:, ft, :], in_=pb[:, 0:TM], func=AF.Copy)
                    I("act", "activation", R=[Rp] + P, W=[Rn("cvb")], out=cvb, in_=pb[:, 0:TM], func=AF.Identity,
                      bias=cb[:, ft:ft + 1], scale=cw[:, ft, 3:4])
                    for sh in (1, 2, 3):
                        I("dve", "scalar_tensor_tensor", R=[Rp] + P, W=[Rn("cvb")], out=cvb[:, sh:TM], in0=pb[:, 0:TM - sh],
                          scalar=cw[:, ft, 3 - sh:4 - sh], in1=cvb[:, sh:TM], op0=ALU.mult, op1=ALU.add)
                    for sh in (1, 2, 3):
                        I("dve", "scalar_tensor_tensor", R=[Rn("hist")] + P, W=[Rn("cvb")], out=cvb[:, 0:sh],
                          in0=hist[:, ft, 3 - sh:3], scalar=cw[:, ft, 3 - sh:4 - sh], in1=cvb[:, 0:sh], op0=ALU.mult, op1=ALU.add)
                    I("act", "activation", R=[Rp, Rn("cvb")], W=[Rn("hist")], out=hist[:, ft, :], in_=pb[:, TM - 3:TM], func=AF.Copy)
                    I("act", "activation", R=[Rn("cvb")], W=[Rn("xcT")], out=xcT[:, ft, :], in_=cvb, func=AF.Silu)
                else:
                    ft = fo - 16
                    I("act", "activation", R=[Rp], W=[Rn("ogT")], out=ogT[:, ft, :], in_=pb[:, 0:TM], func=AF.Sigmoid)
            if stage < 2:
                continue
            for W, src, dst, nm in ((Wq, xcT, qT, "qT"), (Wk, xcT, kT, "kT"), (Wv, xmT, vT, "vT")):
                for g4 in range(4):
                    pb, Rp = self.psum[g4 % 2], self.R_ps[g4 % 2]
                    for j in range(4):
                        ft = 4 * g4 + j
                        I("pe", "matmul", R=[Rn("Wbd"), Rn("xcT"), Rn("xmT")], W=[Rp], out=pb[:, j * TM:(j + 1) * TM],
                          lhsT=W[:, ft, :], rhs=src[:, ft, :], start=True, stop=True)
                    I("act" if g4 % 2 == 0 else "dve", "activation" if g4 % 2 == 0 else "tensor_copy", R=[Rp], W=[Rn(nm)],
                      **(dict(out=dst[:, 4 * g4:4 * g4 + 4, :], in_=pb[:, 0:4 * TM].rearrange("p (j t) -> p j t", j=4), func=AF.Copy)
                         if g4 % 2 == 0 else
                         dict(out=dst[:, 4 * g4:4 * g4 + 4, :], in_=pb[:, 0:4 * TM].rearrange("p (j t) -> p j t", j=4))))
            pg, Rpg = self.psum[3], self.R_ps[3]
            cnt = 0
            for si, src, nm in ((0, qT, "qT"), (1, kT, "kT"), (2, vT, "vT")):
                for ft in range(16):
                    I("pe", "matmul", R=[Rn(nm)] + P, W=[Rpg], out=pg[0:64, 0:8], lhsT=src[:, ft, :], rhs=wgt[:, si, ft, :],
                      start=(cnt == 0), stop=(cnt == 47))
                    cnt += 1
            Rg = Rn("gsm")
            I("dve", "tensor_tensor", R=[Rpg] + P, W=[Rg], out=g_tm[0:64, :], in0=pg[0:64, 0:8], in1=bg_bc[0:64, :], op=ALU.add)
            I("act", "activation", R=[Rg], W=[Rn("lpos")], out=lpos[0:64, :], in_=g_tm[0:64, 4:8], func=AF.Exp, scale=-1.0)
            I("act", "activation", R=[Rn("lpos"), Rc], W=[Rn("lpos")], out=lpos[0:64, :], in_=lpos[0:64, :], func=AF.Ln,
              bias=c[0:64, 130:131])
            I("pe", "matmul", R=[Rn("lpos"), Rc], W=[Rpg], out=pg[0:64, 16:20], lhsT=tri, rhs=lpos[0:64, :], start=True, stop=True)
            I("dve", "tensor_tensor", R=[Rpg, Rg], W=[Rn("a_tm")], out=a_tm[0:64, :], in0=pg[0:64, 16:20], in1=g_tm[0:64, 0:4],
              op=ALU.add)
            I("pe", "matmul", R=[Rn("a_tm"), Rc], W=[Rpg], out=pg[0:4, 32:96], lhsT=a_tm[0:64, :], rhs=ident[0:64, 0:64],
              start=True, stop=True)
            I("pe", "matmul", R=[Rn("lpos"), Rc], W=[Rpg], out=pg[0:4, 96:160], lhsT=lpos[0:64, :], rhs=tri, start=True, stop=True)
            Rrow = Rn("rows")
            I("act", "activation", R=[Rpg], W=[Rrow], out=a_row[0:4, :], in_=pg[0:4, 32:96], func=AF.Copy)
            I("act", "activation", R=[Rpg], W=[Rrow], out=F_row[0:4, :], in_=pg[0:4, 96:160], func=AF.Copy)
            I("dve", "tensor_tensor_scan", R=[Rrow, Rn("mrow"), Rc], W=[Rn("M_row")], out=M_row[0:4, :], data0=c[0:4, 1792:1856],
              data1=a_row[0:4, :], initial=mrow[0:4, 0:1], op0=ALU.mult, op1=ALU.max)
            I("act", "activation", R=[Rn("M_row"), Rn("mrow")], W=[Rn("wi_row")], out=wi_row[0:4, :], in_=M_row[0:4, :], func=AF.Exp,
              scale=-1.0, bias=mrow[0:4, 0:1])
            I("dve", "tensor_tensor", R=[Rrow, Rn("M_row")], W=[Rn("em_row")], out=em_row[0:4, :], in0=F_row[0:4, :], in1=M_row[0:4, :],
              op=ALU.subtract)
            I("act", "activation", R=[Rn("em_row")], W=[Rn("em_row")], out=em_row[0:4, :], in_=em_row[0:4, :], func=AF.Exp)
            I("dve", "tensor_scalar", R=[Rn("M_row")], W=[Rn("negM")], out=negM[0:4, :], in0=M_row[0:4, 63:64], scalar1=-1.0,
              scalar2=None, op0=ALU.mult)
            I("act", "activation", R=[Rrow, Rn("negM")], W=[Rn("wu_row")], out=wu_row[0:4, :], in_=a_row[0:4, :], func=AF.Exp,
              bias=negM[0:4, 0:1])
            I("dve", "tensor_scalar", R=[Rn("wi_row"), Rc], W=[Rn("diagd")], out=diagd[0:4, :], in0=ident[0:4, 0:4],
              scalar1=wi_row[0:4, 63:64], scalar2=None, op0=ALU.mult)
            I("dve", "tensor_tensor", R=[Rn("M_row"), Rrow, Rn("wi_row")], W=[Rn("mrow")], out=mrow[0:4, 0:1], in0=M_row[0:4, 63:64],
              in1=F_row[0:4, 63:64], op=ALU.subtract)
            for qi, (row, nm) in enumerate(((wi_row, "wi_row"), (em_row, "em_row"), (wu_row, "wu_row"))):
                I("pe", "matmul", R=[Rn(nm), Rc], W=[Rpg], out=pg[0:64, 160 + 4 * qi:164 + 4 * qi], lhsT=row[0:4, :],
                  rhs=ident[0:4, 0:4], start=True, stop=True)
            I("pe", "matmul", R=[Rn("diagd"), Rc], W=[Rpg], out=pg[:, 176:180], lhsT=c[0:4, 1856:1984], rhs=diagd[0:4, :],
              start=True, stop=True)
            I("act", "activation", R=[Rpg], W=[Rn("pk")], out=pk[0:64, :], in_=pg[0:64, 160:172], func=AF.Copy)
            I("act", "activation", R=[Rpg], W=[Rn("dec_bc")], out=dec_bc, in_=pg[:, 176:180], func=AF.Copy)
            I("dve", "tensor_scalar", R=[Rn("pk")], W=[Rn("sclk")], out=sclk[0:64, :], in0=pk[0:64, 8:12], scalar1=DHS, scalar2=None,
              op0=ALU.mult)
            ps_s, Rs_ = self.psum[2], self.R_ps[2]
            for h in range(4):
                I("pe", "matmul", R=[Rn("M_row"), Rc], W=[Rs_], out=ps_s[0:64, 256 + h * 64:256 + (h + 1) * 64],
                  lhsT=c[0:4, 1536 + h * 64:1536 + (h + 1) * 64], rhs=M_row[0:4, :], start=True, stop=True)
            if stage < 3:
                continue
            for h in range(4):
                pb, Rp = self.psum[h % 2], self.R_ps[h % 2]
                for j in range(4):
                    ft = 4 * h + j
                    I("pe", "matmul", R=[Rn("xcT"), Rn("Wbd")], W=[Rp], out=pb[0:64, j * 128:(j + 1) * 128], lhsT=xcT[:, ft, :],
                      rhs=Wk[:, ft, :], start=True, stop=True)
                I("act", "activation", R=[Rp, Rn("sclk")], W=[Rn("k_tm")], out=k_tm[0:64, h * 512:(h + 1) * 512], in_=pb[0:64, :],
                  func=AF.Copy, scale=sclk[0:64, h:h + 1])
            for h in range(4):
                pb, Rp = self.psum[h % 2], self.R_ps[h % 2]
                for j in range(4):
                    ft = 4 * h + j
                    I("pe", "matmul", R=[Rn("xmT"), Rn("Wbd")], W=[Rp], out=pb[0:64, j * 128:(j + 1) * 128], lhsT=xmT[:, ft, :],
                      rhs=Wv[:, ft, :], start=True, stop=True)
                I("dve", "tensor_copy", R=[Rp], W=[Rn("v_tm")], out=v_tm[0:64, h * 512:(h + 1) * 512], in_=pb[0:64, :])
            for h in range(4):
                for dt in range(4):
                    I("pe", "matmul", R=[Rn("kT"), Rn("qT")], W=[Rs_], out=ps_s[0:64, h * 64:(h + 1) * 64], lhsT=kT[:, 4 * h + dt, :],
                      rhs=qT[:, 4 * h + dt, :], start=(dt == 0), stop=(dt == 3))
            for h in range(4):
                I("act", "activation", R=[Rs_, Rn("a_tm")], W=[Rn("E")], out=E[0:64, h * 64:(h + 1) * 64],
                  in_=ps_s[0:64, 256 + h * 64:256 + (h + 1) * 64], func=AF.Exp, scale=-1.0, bias=a_tm[0:64, h:h + 1])
            I("dve", "scalar_tensor_tensor", R=[Rs_, Rn("E")], W=[Rn("sTf")], out=sTf[0:64, :], in0=ps_s[0:64, 0:256], scalar=DHS,
              in1=E[0:64, :], op0=ALU.mult, op1=ALU.mult)
            I("pool", "tensor_tensor", R=[Rn("sTf"), Rc], W=[Rn("sT")], out=sT[0:64, :].rearrange("p (h i) -> p h i", h=4),
              in0=sTf[0:64, :].rearrange("p (h i) -> p h i", h=4), in1=self.bc(maskT, 4, pos=0), op=ALU.mult)
            if stage < 4:
                continue
            psm, Rsm = self.psum[3], self.R_ps[3]
            for h in range(4):
                for dt in range(4):
                    I("act" if dt % 2 == 0 else "pool", "activation" if dt % 2 == 0 else "tensor_copy", R=[Rn("Cst")], W=[Rn("Cbf")],
                      **(dict(out=Cbf[:, dt, :], in_=Cst[:, 4 * h + dt, :], func=AF.Copy) if dt % 2 == 0 else
                         dict(out=Cbf[:, dt, :], in_=Cst[:, 4 * h + dt, :])))
                p_i, Rp_i = self.psum[4], self.R_ps[4]
                p_e, Rp_e = self.psum[5], self.R_ps[5]
                I("pe", "matmul", R=[Rn("sT"), Rn("v_tm")], W=[Rp_i], out=p_i[0:64, :], lhsT=sT[0:64, h * 64:(h + 1) * 64],
                  rhs=v_tm[0:64, h * 512:(h + 1) * 512], start=True, stop=True)
                I("pe", "matmul", R=[Rn("sT"), Rn("onesb")], W=[Rsm], out=psm[0:64, 192 + h:193 + h], lhsT=sT[0:64, h * 64:(h + 1) * 64],
                  rhs=onesb[0:64, 0:1], start=True, stop=True)
                for dt in range(4):
                    I("pe", "matmul", R=[Rn("qT"), Rn("Cbf")], W=[Rp_e], out=p_e[0:64, :], lhsT=qT[:, 4 * h + dt, :], rhs=Cbf[:, dt, :],
                      start=(dt == 0), stop=(dt == 3))
                for dt in range(4):
                    I("pe", "matmul", R=[Rn("qT"), Rn("nbf")], W=[Rsm], out=psm[0:64, 196 + h:197 + h], lhsT=qT[:, 4 * h + dt, :],
                      rhs=nbf[:, 4 * h + dt:4 * h + dt + 1], start=(dt == 0), stop=(dt == 3))
                Rd = Rn("dens")
                I("act", "activation", R=[Rsm], W=[Rd], out=dint[0:64, h:h + 1], in_=psm[0:64, 192 + h:193 + h], func=AF.Copy)
                I("dve", "scalar_tensor_tensor", R=[Rsm, Rn("pk"), Rd], W=[Rd], out=den[0:64, h:h + 1], in0=psm[0:64, 196 + h:197 + h],
                  scalar=pk[0:64, h:h + 1], in1=dint[0:64, h:h + 1], op0=ALU.mult, op1=ALU.add)
                I("dve", "tensor_scalar", R=[Rd], W=[Rd], out=nden[0:64, h:h + 1], in0=den[0:64, h:h + 1], scalar1=-1.0, scalar2=None,
                  op0=ALU.mult)
                I("dve", "tensor_tensor", R=[Rd], W=[Rd], out=den[0:64, h:h + 1], in0=den[0:64, h:h + 1], in1=nden[0:64, h:h + 1],
                  op=ALU.max)
                I("dve", "tensor_tensor", R=[Rd, Rn("pk")], W=[Rd], out=den[0:64, h:h + 1], in0=den[0:64, h:h + 1],
                  in1=pk[0:64, 4 + h:5 + h], op=ALU.max)
                I("dve", "reciprocal", R=[Rd], W=[Rd], out=rden[0:64, h:h + 1], in_=den[0:64, h:h + 1])
                I("dve", "tensor_tensor", R=[Rd, Rn("pk")], W=[Rd], out=wr[0:64, h:h + 1], in0=rden[0:64, h:h + 1], in1=pk[0:64, h:h + 1],
                  op=ALU.mult)
                I("act", "activation", R=[Rp_i, Rd], W=[Rn("t1")], out=t1[0:64, :], in_=p_i[0:64, :], func=AF.Copy,
                  scale=rden[0:64, h:h + 1])
                I("dve", "scalar_tensor_tensor", R=[Rp_e, Rd, Rn("t1")], W=[Rn("hh")], out=hh[0:64, :], in0=p_e[0:64, :],
                  scalar=wr[0:64, h:h + 1], in1=t1[0:64, :], op0=ALU.mult, op1=ALU.add)
                I("act", "activation", R=[Rn("hh")], W=[Rn("t1"), Rn("ss")], out=t1[0:64, :], in_=hh[0:64, :], func=AF.Square,
                  accum_out=ss[0:64, h:h + 1])
                I("act", "activation", R=[Rn("ss"), Rc], W=[Rn("rs")], out=rs[0:64, h:h + 1], in_=ss[0:64, h:h + 1], func=AF.Sqrt,
                  scale=1.0 / 512, bias=c[0:64, 128:129])
                I("dve", "reciprocal", R=[Rn("rs")], W=[Rn("rs")], out=rs[0:64, h:h + 1], in_=rs[0:64, h:h + 1])
                I("act", "activation", R=[Rn("hh"), Rn("rs")], W=[Rn("t1")], out=t1[0:64, :], in_=hh[0:64, :], func=AF.Copy,
                  scale=rs[0:64, h:h + 1])
                p_t, Rp_t = self.psum[6], self.R_ps[6]
                for dt in range(4):
                    I("pe", "transpose", R=[Rn("t1"), Rc], W=[Rp_t], out=p_t[:, dt * 64:(dt + 1) * 64],
                      in_=t1[0:64, dt * 128:(dt + 1) * 128], identity=ident[0:64, 0:64])
                fs = slice(4 * h, 4 * h + 4)
                I("pool", "tensor_tensor", R=[Rn("xcT")] + P, W=[Rn("u1")], out=u1, in0=xcT[:, fs, :], in1=self.bc(sk[:, fs], 64),
                  op=ALU.mult)
                I("dve", "tensor_tensor", R=[Rp_t] + P, W=[Rn("u2")], out=u2, in0=p_t[:, 0:256].rearrange("p (j t) -> p j t", j=4),
                  in1=self.bc(ng[:, fs], 64), op=ALU.mult)
                I("pool", "tensor_tensor", R=[Rn("u1"), Rn("u2")], W=[Rn("u2")], out=u2, in0=u2, in1=u1, op=ALU.add)
                I("pool", "tensor_tensor", R=[Rn("u2"), Rn("ogT")], W=[Rn("ogT")], out=ogT[:, fs, :], in0=u2, in1=ogT[:, fs, :],
                  op=ALU.mult)
                for dt in range(4):
                    p_c, Rp_c = self.psum[dt % 2], self.R_ps[dt % 2]
                    I("pe", "matmul", R=[Rn("k_tm"), Rn("v_tm")], W=[Rp_c], out=p_c[:, :],
                      lhsT=k_tm[0:64, (4 * h + dt) * 128:(4 * h + dt + 1) * 128], rhs=v_tm[0:64, h * 512:(h + 1) * 512],
                      start=True, stop=True)
                    I("dve", "scalar_tensor_tensor", R=[Rp_c, Rn("dec_bc"), Rn("Cbf")], W=[Rn("Cst")], out=Cst[:, 4 * h + dt, :],
                      in0=Cst[:, 4 * h + dt, :], scalar=dec_bc[:, h:h + 1], in1=p_c[:, :], op0=ALU.mult, op1=ALU.add)
                    I("pe", "matmul", R=[Rn("k_tm"), Rn("onesb")], W=[Rsm], out=psm[:, 208 + 4 * h + dt:209 + 4 * h + dt],
                      lhsT=k_tm[0:64, (4 * h + dt) * 128:(4 * h + dt + 1) * 128], rhs=onesb[0:64, 0:1], start=True, stop=True)
            I("dve", "tensor_tensor", R=[Rn("dec_bc"), Rn("nbf")], W=[Rn("nst")], out=nst.rearrange("p (h d) -> p h d", h=4),
              in0=nst.rearrange("p (h d) -> p h d", h=4), in1=self.bc(dec_bc, 4), op=ALU.mult)
            I("dve", "tensor_tensor", R=[Rsm], W=[Rn("nst")], out=nst, in0=nst, in1=psm[:, 208:224], op=ALU.add)
            I("act", "activation", R=[Rn("nst")], W=[Rn("nbf")], out=nbf, in_=nst, func=AF.Copy)
            if stage < 5:
                continue
            R_f = Rn("fbuf")
            for ob in range(8):
                pb, Rp = self.psum[4 + ob % 2], self.R_ps[4 + ob % 2]
                for hf in range(2):
                    w, Rw = self.load_w(sc_oout[o, ob, hf], R_scr)
                    for k in range(8):
                        I("pe", "matmul", R=[Rw, Rn("ogT")], W=[Rp], out=pb[:, 0:TM], lhsT=w[:, k, :], rhs=ogT[:, hf * 8 + k, :],
                          start=(hf == 0 and k == 0), stop=(hf == 1 and k == 7))
                I("act", "activation", R=[Rp], W=[R_f], out=self.fbuf[:, ob, 0:TM], in_=pb[:, 0:TM], func=AF.Copy)
            self.epilogue(None, self.lnw["ln_mix_post"], l, t0=t0, n=TM)


def make_consts():
    c = np.zeros((128, 2048), np.float32)
    c[:, 0:128] = np.eye(128, dtype=np.float32)
    c[:, 128] = EPS
    c[:, 129] = np.pi / 2
    c[:, 130] = 1.0
    p = np.arange(128)
    for g in range(8):
        c[:, 136 + g] = (p // 16 == g)
    for g in range(4):
        c[:, 144 + g] = (p % 4 == g)
    j = np.arange(64)[:, None]
    i = np.arange(64)[None, :]
    c[0:64, 256:320] = (j <= i)
    c[64:128, 256:320] = (j <= i)
    c[0:64, 320:384] = (j <= i)
    c[:, 512:1024] = np.arange(512, dtype=np.float32)[None, :]
    cm = np.ones(512, np.float32)
    cm[::64] = 0
    c[:, 1024:1536] = cm[None, :]
    cc = np.arange(128)
    c[:, 384:512] = (p[:, None] // 4 == cc[None, :] // 4)
    for h in range(4):
        c[h, 1536 + h * 64:1536 + (h + 1) * 64] = 1.0
    c[:, 1792:1984] = 1.0
    return c


_CACHE = {}


def run_module(inputs, nseq, layers, parts, batch_sel=None):
    key = (nseq, tuple(layers), tuple(parts))
    if key not in _CACHE:
        b = Builder(nseq, layers, parts)
        _CACHE[key] = (b.build(), list(b.drams.keys()))
    nc, nc_inputs = _CACHE[key]
    x = np.asarray(inputs["x"], dtype=np.float32)
    in_maps = []
    for c in range(NCORES):
        m = {}
        if batch_sel is None:
            m["x"] = np.ascontiguousarray(x[c * nseq:(c + 1) * nseq])
        else:
            m["x"] = np.ascontiguousarray(x[batch_sel[c]])
        for k in nc_inputs:
            if k not in ("x", "consts"):
                m[k] = np.ascontiguousarray(np.asarray(inputs[k], dtype=np.float32))
        m["consts"] = make_consts()
        in_maps.append(m)
    res = run_bass_kernel_spmd(nc, in_maps, core_ids=list(range(NCORES)))
    return np.concatenate([r["y"] for r in res.results], axis=0)


def kernel(**inputs):
    mode = MODE
    x = np.asarray(inputs["x"], dtype=np.float32)
    if mode == "fused":
        return run_module(inputs, NB // NCORES, list(range(DEPTH)), ("mix", "ffn")).astype(np.float32)
    out = np.empty_like(x)
    per = NB // NCORES
    for s_ in range(per):
        sel = [[c * per + s_] for c in range(NCORES)]
        y = run_module(inputs, 1, list(range(DEPTH)), ("mix", "ffn"), batch_sel=sel)
        for c in range(NCORES):
            out[c * per + s_] = y[c]
    return out


MODE = "unfused"
```

```python
import numpy as np
from contextlib import ExitStack
import concourse.bass as bass
import concourse.mybir as mybir
from concourse.bass_utils import run_bass_kernel_spmd

F32 = mybir.dt.float32
BF16 = mybir.dt.bfloat16
AF = mybir.ActivationFunctionType
ALU = mybir.AluOpType

D = 1024
L = 2048
NB = 32
DEPTH = 4
FF = 2816
NHB = FF // 128
TT = 512
NTT = L // TT
EPS = 1e-6
NCORES = 8


class Res:
    __slots__ = ("name", "w", "r")

    def __init__(self, name):
        self.name = name
        self.w = None
        self.r = []


class Sched:
    EPOCH = 20000

    def __init__(self, nc, stack):
        self.nc = nc
        self.stack = stack
        self.names = ["pe", "act", "dve", "pool", "sp"]
        self.prog = {e: [] for e in self.names}
        self.cnt = {e: 0 for e in self.names}
        self.seen = {e: {} for e in self.names}
        self.sems = {}
        self.dcnt = {}
        self.self_sync = {"pe": False, "act": True, "dve": True, "pool": True, "sp": False}

    def sem(self, key):
        if key not in self.sems:
            nm = "s_" + "_".join(str(k) for k in key)
            self.sems[key] = self.stack.enter_context(self.nc.semaphore(nm))
        return self.sems[key]

    def _waits(self, eng, reads, writes):
        need = {}
        for t in reads:
            if t.w is not None:
                k, v = t.w
                need[k] = max(need.get(k, 0), v)
        for t in writes:
            if t.w is not None:
                k, v = t.w
                need[k] = max(need.get(k, 0), v)
            for (k, v) in t.r:
                need[k] = max(need.get(k, 0), v)
        waits = []
        for k, v in need.items():
            if self.seen[eng].get(k, 0) < v:
                waits.append((k, v))
                self.seen[eng][k] = v
        return waits

    def _commit(self, ev, reads, writes):
        for t in reads:
            t.r.append(ev)
            if len(t.r) > 64:
                mx = {}
                for k, v in t.r:
                    mx[k] = max(mx.get(k, 0), v)
                t.r = list(mx.items())
        for t in writes:
            t.w = ev
            t.r = []

    def op(self, eng, fn, reads=(), writes=()):
        waits = self._waits(eng, reads, writes)
        n = self.cnt[eng]
        self.cnt[eng] += 1
        key = ("e", eng, n // self.EPOCH)
        val = n % self.EPOCH + 1
        self.sem(key)
        for k, _ in waits:
            self.sem(k)
        self.prog[eng].append((waits, fn, (key, 1)))
        if not self.self_sync[eng]:
            self.seen[eng][key] = val
        self._commit((key, val), reads, writes)

    def dma(self, q, fn, reads=(), writes=(), semkey=None):
        waits = self._waits(q, reads, writes)
        key = ("d", semkey)
        self.dcnt[key] = self.dcnt.get(key, 0) + 1
        val = 16 * self.dcnt[key]
        self.sem(key)
        for k, _ in waits:
            self.sem(k)
        self.prog[q].append((waits, fn, (key, 16)))
        self._commit((key, val), reads, writes)
        return (key, val)

    def wait_all(self, eng, events):
        waits = []
        for k, v in events:
            if self.seen[eng].get(k, 0) < v:
                waits.append((k, v))
                self.seen[eng][k] = v
        self.prog[eng].append((waits, None, None))

    def replay(self, eng, e):
        for waits, fn, inc in self.prog[eng]:
            for k, v in waits:
                e.wait_ge(self.sems[k], v)
            if fn is None:
                continue
            ins = fn(e)
            if inc is not None:
                ins.then_inc(self.sems[inc[0]], inc[1])


class Builder:
    def __init__(self, nseq, layers, parts, dbg=False):
        self.nseq = nseq
        self.layers = layers
        self.parts = parts
        self.nc = bass.Bass("TRN2", target_bir_lowering=False)
        self.stack = ExitStack()
        self.S = Sched(self.nc, self.stack)
        self.res = {}
        self.drams = {}

    def sb(self, name, shape, dt=F32):
        t = self.stack.enter_context(self.nc.sbuf_tensor(name, list(shape), dt))
        return t

    def R(self, name):
        if name not in self.res:
            self.res[name] = Res(name)
        return self.res[name]

    def I(self, eng, meth, R=(), W=(), **kw):
        self.S.op(eng, (lambda e: getattr(e, meth)(**kw)), reads=R, writes=W)

    def carve_reset(self):
        self.aoff = 0

    def carve(self, shape, dt=F32):
        n = int(np.prod(shape))
        nb = n if dt == BF16 else 2 * n
        nb = (nb + 31) // 32 * 32
        a = self.arena[:, self.aoff:self.aoff + nb]
        self.aoff += nb
        assert self.aoff <= self.ARENA, (self.aoff, self.ARENA)
        if dt != BF16:
            a = a.bitcast(dt)
        a = a[:, 0:n]
        if len(shape) == 2:
            a = a.rearrange("p (a b) -> p a b", a=shape[0])
        elif len(shape) == 3:
            a = a.rearrange("p (a b c) -> p a b c", a=shape[0], b=shape[1])
        return a

    def barrier(self):
        S = self.S
        evs = []
        for eng in S.names:
            n = S.cnt[eng]
            if n > 0:
                evs.append((("e", eng, (n - 1) // S.EPOCH), (n - 1) % S.EPOCH + 1))
        for k, n in S.dcnt.items():
            evs.append((k, 16 * n))
        for eng in S.names:
            S.wait_all(eng, evs)

    @staticmethod
    def bc(ap, n, pos=None):
        lst = [list(x) for x in ap.ap]
        if pos is None:
            lst.append([0, n])
        else:
            lst.insert(1 + pos, [0, n])
        return bass.AP(ap.tensor, ap.offset, lst)

    def din(self, name, shape, dt=F32):
        h = self.nc.dram_tensor(name, list(shape), dt, kind="ExternalInput")
        self.drams[name] = h
        return h.ap()

    def dscratch(self, name, shape, dt=BF16):
        h = self.nc.dram_tensor(name, list(shape), dt, kind="Internal")
        return h.ap()

    def build(self):
        nc, S = self.nc, self.S
        nseq = self.nseq
        x_in = self.din("x", [nseq, L, D])
        y_out = nc.dram_tensor("y", [nseq, L, D], F32, kind="ExternalOutput").ap()
        ln = {k: self.din(k, [DEPTH, D]) for k in ("ln_mix_pre", "ln_mix_post", "ln_ffn_pre", "ln_ffn_post")}
        ffn_w_up = self.din("ffn_w_up", [DEPTH, D, 2 * FF])
        ffn_conv_w = self.din("ffn_conv_w", [DEPTH, 3, FF])
        ffn_conv_b = self.din("ffn_conv_b", [DEPTH, FF])
        ffn_w_down = self.din("ffn_w_down", [DEPTH, FF, D])
        consts = self.din("consts", [128, 2048])
        EV = {"ev_w_in": [2, D, 2064], "s5_lambda_re": [2, 32, 64], "s5_lambda_im": [2, 32, 64], "s5_b_re": [2, 32, 64, 16],
              "s5_b_im": [2, 32, 64, 16], "s5_c_re": [2, 32, 16, 64], "s5_c_im": [2, 32, 16, 64], "s5_d": [2, 32, 16],
              "s5_log_dt": [2, 32], "s5_w_glu": [2, 512, 512], "s5_b_glu": [2, 512], "gla_w_alpha_up": [2, 16, 256],
              "gla_b_alpha": [2, 256], "gla_norm": [2, 512], "ev_w_out": [2, D, D]}
        OD = {"od_w_in": [2, D, 4096], "ml_conv_w": [2, 4, 2048], "ml_conv_b": [2, 2048], "ml_w_q": [2, 512, 4, 4],
              "ml_w_k": [2, 512, 4, 4], "ml_w_v": [2, 512, 4, 4], "ml_w_gate": [2, 3, 2048, 8], "ml_b_gate": [2, 8],
              "ml_norm": [2, 2048], "ml_skip": [2, 2048], "od_w_out": [2, 2048, D]}
        self.inp = {}
        has_even = "mix" in self.parts and any(l % 2 == 0 for l in self.layers)
        has_odd = "mix" in self.parts and any(l % 2 == 1 for l in self.layers)
        if has_even:
            for k, shp in EV.items():
                self.inp[k] = self.din(k, shp)
        if has_odd:
            for k, shp in OD.items():
                self.inp[k] = self.din(k, shp)
        self.consts_dram = consts

        sc_up = self.dscratch("sc_up", [DEPTH, 2, NHB, 128, 8, 128])
        sc_dn = self.dscratch("sc_dn", [DEPTH, NHB, 128, 8, 128])
        self.prologue_events = []
        R_scr = self.R("scratch_w")

        used_layers = sorted(set(self.layers))
        if "ffn" in self.parts:
            for l in used_layers:
                for gu in range(2):
                    for hb in range(NHB):
                        c0 = gu * FF + hb * 128
                        src = ffn_w_up[l, :, c0:c0 + 128].rearrange("(kt p) c -> p kt c", p=128)
                        dst = sc_up[l, gu, hb]
                        ev = S.dma("pool", (lambda e, dst=dst, src=src: e.dma_start(out=dst, in_=src)),
                                   writes=[], semkey="prolog")
                        self.prologue_events.append(ev)
                src = ffn_w_down[l].rearrange("(hb p) (ob c) -> hb p ob c", p=128, c=128)
                for hb in range(NHB):
                    ev = S.dma("pool", (lambda e, dst=sc_dn[l, hb], src=src[hb]: e.dma_start(out=dst, in_=src)),
                               writes=[], semkey="prolog")
                    self.prologue_events.append(ev)
        if has_even:
            self.sc_ein = self.dscratch("sc_ein", [2, 16, 128, 8, 128])
            self.sc_eout = self.dscratch("sc_eout", [2, 8, 128, 8, 128])
            for e_ in sorted(set(l // 2 for l in used_layers if l % 2 == 0)):
                for blk in range(16):
                    src = self.inp["ev_w_in"][e_, :, blk * 128:(blk + 1) * 128].rearrange("(kt p) c -> p kt c", p=128)
                    S.dma("pool", (lambda e, dst=self.sc_ein[e_, blk], src=src: e.dma_start(out=dst, in_=src)), semkey="prolog")
                for ob in range(8):
                    src = self.inp["ev_w_out"][e_, :, ob * 128:(ob + 1) * 128].rearrange("(kt p) c -> p kt c", p=128)
                    S.dma("pool", (lambda e, dst=self.sc_eout[e_, ob], src=src: e.dma_start(out=dst, in_=src)), semkey="prolog")
        if has_odd:
            self.prologue_odd(used_layers)
        R_scr.w = (("d", "prolog"), 16 * S.dcnt.get(("d", "prolog"), 0)) if S.dcnt.get(("d", "prolog"), 0) else None

        self.x_res = self.sb("x_res", [128, 8, L], F32)
        self.R_x = [self.R(f"x_{tt}") for tt in range(NTT)]
        self.hn = self.sb("hn", [128, 8, TT], BF16)
        self.sq = [self.sb(f"sq{i}", [128, TT], BF16) for i in range(2)]
        self.rstd = self.sb("rstd", [128, TT], F32)
        self.fbuf = self.sb("fbuf", [128, 8, TT], F32)
        self.tmpf = [self.sb(f"tmpf{i}", [128, TT], F32) for i in range(2)]
        self.cst = self.sb("cst", [128, 2048], F32)
        self.ones_bf = self.sb("ones_bf", [128, 128], BF16)
        self.lnw = {k: self.sb("w_" + k, [128, DEPTH, 8], F32) for k in ln}
        self.psum = [self.stack.enter_context(nc.psum_tensor(f"ps{i}", [128, 512], F32)) for i in range(8)]
        self.R_ps = [self.R(f"ps{i}") for i in range(8)]
        self.NSLOT = 6
        self.wslot = [self.sb(f"wslot{i}", [128, 8, 128], BF16) for i in range(self.NSLOT)]
        self.R_wslot = [self.R(f"wslot{i}") for i in range(self.NSLOT)]
        self.slot_i = 0

        self.ARENA = 45440
        self.arena = self.sb("arena", [128, self.ARENA], BF16)
        self.R_hid = [self.R(f"hid{i}") for i in range(NHB)]
        self.fcw = self.sb("fcw", [128, DEPTH, 3, NHB], F32)
        self.fcb = self.sb("fcb", [128, DEPTH, NHB], F32)

        R_c = self.R("consts")
        S.dma("sp", lambda e: e.dma_start(out=self.cst[:], in_=consts[:, :]), semkey="setup")
        for k in ln:
            S.dma("sp", (lambda e, k=k: e.dma_start(out=self.lnw[k][:], in_=ln[k].rearrange("l (t p) -> p l t", p=128),
                                                    allow_slow_non_contiguous=True)),
                  semkey="setup")
        S.dma("sp", lambda e: e.dma_start(out=self.fcw[:], in_=ffn_conv_w.rearrange("l k (t p) -> p l k t", p=128),
                                          allow_slow_non_contiguous=True), semkey="setup")
        S.dma("sp", lambda e: e.dma_start(out=self.fcb[:], in_=ffn_conv_b.rearrange("l (t p) -> p l t", p=128),
                                          allow_slow_non_contiguous=True), semkey="setup")
        self.R_c = R_c
        self.setup_extra()
        R_c.w = (("d", "setup"), 16 * S.dcnt[("d", "setup")])
        S.op("dve", lambda e: e.memset(self.ones_bf[:], 1.0 / D), writes=[self.R("ones")])

        out_events = []
        for s in range(nseq):
            self.load_x(x_in, s)
            for l in self.layers:
                if "mix" in self.parts:
                    if l % 2 == 0:
                        self.mix_even(l)
                    else:
                        self.mix_odd(l)
                if "ffn" in self.parts:
                    self.ffn_layer(l, sc_up, sc_dn, R_scr)
            out_events += self.store_x(y_out, s)
        S.wait_all("sp", out_events)

        with nc.Block() as block:
            @block.tensor
            def _(e):
                S.replay("pe", e)

            @block.scalar
            def _(e):
                S.replay("act", e)

            @block.vector
            def _(e):
                S.replay("dve", e)

            @block.gpsimd
            def _(e):
                S.replay("pool", e)

            @block.sync
            def _(e):
                S.replay("sp", e)
        self.stack.close()
        return nc

    def load_x(self, x_in, s):
        S = self.S
        ident = self.cst[:, 0:128]
        for tt in range(NTT):
            for sub in range(TT // 128):
                t0 = tt * TT + sub * 128
                stg = self.tmpf
                R_stage = self.R("fbuf")
                S.dma("sp", (lambda e, t0=t0: e.dma_start(out=self.fbuf[:, 0:2, :].rearrange("p a b -> p (a b)"),
                                                          in_=x_in[s, t0:t0 + 128, :])),
                      writes=[R_stage], semkey="xstage")
                for half in range(2):
                    pb = self.psum[half]
                    Rp = self.R_ps[half]
                    for j in range(4):
                        ft = half * 4 + j
                        S.op("pe", (lambda e, ft=ft, j=j, pb=pb: e.transpose(
                            out=pb[:, j * 128:(j + 1) * 128],
                            in_=self.fbuf[:, 0:2, :].rearrange("p a b -> p (a b)")[:, ft * 128:(ft + 1) * 128],
                            identity=ident)), reads=[R_stage, self.R_c], writes=[Rp])
                    eng = "act" if half == 0 else "dve"
                    dst = self.x_res[:, half * 4:(half + 1) * 4, t0:t0 + 128]
                    src = pb[:, :].rearrange("p (j t) -> p j t", j=4)
                    if eng == "act":
                        S.op("act", (lambda e, dst=dst, src=src: e.activation(out=dst, in_=src, func=AF.Copy)),
                             reads=[Rp], writes=[self.R_x[tt]])
                    else:
                        S.op("dve", (lambda e, dst=dst, src=src: e.tensor_copy(out=dst, in_=src)),
                             reads=[Rp], writes=[self.R_x[tt]])

    def store_x(self, y_out, s):
        S = self.S
        ident = self.cst[:, 0:128]
        evs = []
        for tt in range(NTT):
            for sub in range(TT // 128):
                t0 = tt * TT + sub * 128
                R_stage = self.R("fbuf")
                for half in range(2):
                    pb = self.psum[half]
                    Rp = self.R_ps[half]
                    for j in range(4):
                        ft = half * 4 + j
                        S.op("pe", (lambda e, ft=ft, j=j, pb=pb, t0=t0: e.transpose(
                            out=pb[:, j * 128:(j + 1) * 128], in_=self.x_res[:, ft, t0:t0 + 128], identity=ident)),
                             reads=[self.R_x[tt], self.R_c], writes=[Rp])
                    dst = self.fbuf[:, 2 + half, :]
                    if half == 0:
                        S.op("act", (lambda e, dst=dst, pb=pb: e.activation(out=dst, in_=pb[:, :], func=AF.Copy)),
                             reads=[Rp], writes=[R_stage])
                    else:
                        S.op("dve", (lambda e, dst=dst, pb=pb: e.tensor_copy(out=dst, in_=pb[:, :])),
                             reads=[Rp], writes=[R_stage])
                ev = S.dma("sp", (lambda e, t0=t0: e.dma_start(out=y_out[s, t0:t0 + 128, :],
                                                               in_=self.fbuf[:, 2:4, :].rearrange("p a b -> p (a b)"))),
                           reads=[R_stage], semkey="ystore")
                evs.append(ev)
        return evs

    def prenorm(self, tt, gain, l, t0=None, n=TT):
        S = self.S
        if t0 is None:
            t0 = tt * TT
        tt = t0 // TT
        ts = slice(t0, t0 + n)
        Rx = self.R_x[tt]
        R_hn = self.R("hn")
        self.rms_stats([self.x_res[:, k, ts] for k in range(8)], [Rx], self.ones_bf, 8, n=n)
        R_rstd = self.R("rstd")
        for k in range(8):
            S.op("dve", (lambda e, k=k: e.scalar_tensor_tensor(out=self.hn[:, k, 0:n], in0=self.x_res[:, k, ts],
                                                               scalar=gain[:, l, k:k + 1], in1=self.rstd[:, 0:n],
                                                               op0=ALU.mult, op1=ALU.mult)),
                 reads=[Rx, R_rstd, self.R_c], writes=[R_hn])

    def rms_stats(self, srcs, src_res, ones, nt, psi=7, ones_res=None, n=TT, scale=1.0):
        S = self.S
        R_rstd = self.R("rstd")
        Rp = self.R_ps[psi]
        pb = self.psum[psi]
        for k in range(nt):
            Rs = self.R(f"sq{k % 2}")
            S.op("act", (lambda e, k=k: e.activation(out=self.sq[k % 2][:, 0:n], in_=srcs[k], func=AF.Square)),
                 reads=list(src_res), writes=[Rs])
            S.op("pe", (lambda e, k=k: e.matmul(pb[:, 0:n], lhsT=ones[:, :], rhs=self.sq[k % 2][:, 0:n],
                                                start=(k == 0), stop=(k == nt - 1))),
                 reads=[Rs, ones_res or self.R("ones")], writes=[Rp])
        S.op("act", lambda e: e.activation(out=self.rstd[:, 0:n], in_=pb[:, 0:n], func=AF.Sqrt, bias=self.eps_ap(), scale=1.0),
             reads=[Rp, self.R_c], writes=[R_rstd])
        S.op("dve", lambda e: e.reciprocal(out=self.rstd[:, 0:n], in_=self.rstd[:, 0:n]), reads=[R_rstd], writes=[R_rstd])

    def setup_extra(self):
        pass

    def eps_ap(self):
        return self.cst[:, 128:129]

    def epilogue(self, tt, gain, l, t0=None, n=TT):
        S = self.S
        if t0 is None:
            t0 = tt * TT
        tt = t0 // TT
        ts = slice(t0, t0 + n)
        R_f = self.R("fbuf")
        self.rms_stats([self.fbuf[:, k, 0:n] for k in range(8)], [R_f], self.ones_bf, 8, n=n)
        R_rstd = self.R("rstd")
        for k in range(8):
            Rt = self.R(f"tmpf{k % 2}")
            S.op("dve", (lambda e, k=k: e.scalar_tensor_tensor(out=self.tmpf[k % 2][:, 0:n], in0=self.fbuf[:, k, 0:n],
                                                               scalar=gain[:, l, k:k + 1], in1=self.rstd[:, 0:n],
                                                               op0=ALU.mult, op1=ALU.mult)),
                 reads=[R_f, R_rstd, self.R_c], writes=[Rt])
            S.op("pool", (lambda e, k=k: e.tensor_tensor(out=self.x_res[:, k, ts], in0=self.x_res[:, k, ts],
                                                         in1=self.tmpf[k % 2][:, 0:n], op=ALU.add)),
                 reads=[Rt], writes=[self.R_x[tt]])

    def load_w(self, src_ap, R_src):
        S = self.S
        i = self.slot_i
        self.slot_i = (self.slot_i + 1) % self.NSLOT
        q = "sp"
        S.dma(q, (lambda e, i=i, src_ap=src_ap: e.dma_start(out=self.wslot[i][:], in_=src_ap)),
              reads=[R_src], writes=[self.R_wslot[i]], semkey=f"wslot{i}")
        return self.wslot[i], self.R_wslot[i]

    def ffn_layer(self, l, sc_up, sc_dn, R_scr):
        S = self.S
        self.barrier()
        self.carve_reset()
        self.hid = self.carve([NHB, TT], BF16)
        self.gprev = self.carve([NHB, 2], F32)
        self.cv = [self.carve([TT], F32) for i in range(2)]
        self.ge = [self.carve([TT], F32) for i in range(2)]
        S.op("pool", lambda e: e.memset(self.gprev, 0.0), writes=[self.R("gprev")])
        for tt in range(NTT):
            self.prenorm(tt, self.lnw["ln_ffn_pre"], l)
            R_hn = self.R("hn")
            for hb in range(NHB):
                wg, Rwg = self.load_w(sc_up[l, 0, hb], R_scr)
                wu, Rwu = self.load_w(sc_up[l, 1, hb], R_scr)
                pg, Rpg = self.psum[(2 * hb) % 4], self.R_ps[(2 * hb) % 4]
                pu, Rpu = self.psum[(2 * hb) % 4 + 1], self.R_ps[(2 * hb) % 4 + 1]
                for k in range(8):
                    S.op("pe", (lambda e, k=k, wg=wg, pg=pg: e.matmul(pg[:, :], lhsT=wg[:, k, :], rhs=self.hn[:, k, :],
                                                                      start=(k == 0), stop=(k == 7))),
                         reads=[Rwg, R_hn], writes=[Rpg])
                for k in range(8):
                    S.op("pe", (lambda e, k=k, wu=wu, pu=pu: e.matmul(pu[:, :], lhsT=wu[:, k, :], rhs=self.hn[:, k, :],
                                                                      start=(k == 0), stop=(k == 7))),
                         reads=[Rwu, R_hn], writes=[Rpu])
                cv, Rcv = self.cv[hb % 2], self.R(f"cv{hb % 2}")
                ge, Rge = self.ge[hb % 2], self.R(f"ge{hb % 2}")
                w0 = self.fcw[:, l, 0, hb:hb + 1]
                w1 = self.fcw[:, l, 1, hb:hb + 1]
                w2 = self.fcw[:, l, 2, hb:hb + 1]
                bb = self.fcb[:, l, hb:hb + 1]
                Rgp = self.R("gprev")
                S.op("act", (lambda e, cv=cv, pg=pg, w2=w2, bb=bb: e.activation(out=cv[:], in_=pg[:, :], func=AF.Identity,
                                                                                bias=bb, scale=w2)),
                     reads=[Rpg, self.R_c], writes=[Rcv])
                S.op("dve", (lambda e, cv=cv, pg=pg, w1=w1: e.scalar_tensor_tensor(
                    out=cv[:, 1:TT], in0=pg[:, 0:TT - 1], scalar=w1, in1=cv[:, 1:TT], op0=ALU.mult, op1=ALU.add)),
                     reads=[Rpg, self.R_c], writes=[Rcv])
                S.op("dve", (lambda e, cv=cv, pg=pg, w0=w0: e.scalar_tensor_tensor(
                    out=cv[:, 2:TT], in0=pg[:, 0:TT - 2], scalar=w0, in1=cv[:, 2:TT], op0=ALU.mult, op1=ALU.add)),
                     reads=[Rpg, self.R_c], writes=[Rcv])
                S.op("dve", (lambda e, cv=cv, hb=hb, w0=w0: e.scalar_tensor_tensor(
                    out=cv[:, 0:2], in0=self.gprev[:, hb, 0:2], scalar=w0, in1=cv[:, 0:2], op0=ALU.mult, op1=ALU.add)),
                     reads=[Rgp, self.R_c], writes=[Rcv])
                S.op("dve", (lambda e, cv=cv, hb=hb, w1=w1: e.scalar_tensor_tensor(
                    out=cv[:, 0:1], in0=self.gprev[:, hb, 1:2], scalar=w1, in1=cv[:, 0:1], op0=ALU.mult, op1=ALU.add)),
                     reads=[Rgp, self.R_c], writes=[Rcv])
                S.op("act", (lambda e, pg=pg, hb=hb: e.activation(out=self.gprev[:, hb, :], in_=pg[:, TT - 2:TT], func=AF.Copy)),
                     reads=[Rpg, Rcv], writes=[Rgp])
                S.op("act", (lambda e, cv=cv, ge=ge: e.activation(out=ge[:], in_=cv[:], func=AF.Gelu_apprx_tanh)),
                     reads=[Rcv], writes=[Rge])
                S.op("dve", (lambda e, ge=ge, pu=pu, hb=hb: e.tensor_tensor(out=self.hid[:, hb, :], in0=ge[:], in1=pu[:, :],
                                                                            op=ALU.mult)),
                     reads=[Rge, Rpu], writes=[self.R_hid[hb]])
            R_f = self.R("fbuf")
            for hb in range(NHB):
                wd, Rwd = self.load_w(sc_dn[l, hb], R_scr)
                for ob in range(8):
                    S.op("pe", (lambda e, hb=hb, ob=ob, wd=wd: e.matmul(
                        self.psum[ob][:, :], lhsT=wd[:, ob, :], rhs=self.hid[:, hb, :],
                        start=(hb == 0), stop=(hb == NHB - 1))),
                         reads=[Rwd, self.R_hid[hb]], writes=[self.R_ps[ob]])
            for ob in range(8):
                if ob % 2 == 0:
                    S.op("act", (lambda e, ob=ob: e.activation(out=self.fbuf[:, ob, :], in_=self.psum[ob][:, :],
                                                               func=AF.Copy)),
                         reads=[self.R_ps[ob]], writes=[R_f])
                else:
                    S.op("dve", (lambda e, ob=ob: e.tensor_copy(out=self.fbuf[:, ob, :], in_=self.psum[ob][:, :])),
                         reads=[self.R_ps[ob]], writes=[R_f])
            self.epilogue(tt, self.lnw["ln_ffn_post"], l)


    def rr3(self, eng, out, x, tmp, R, W):
        MAGIC = 12582912.0
        self.I(eng, "tensor_scalar", R=R, W=W, out=tmp, in0=x, scalar1=float(1.0 / (2 * np.pi)), scalar2=MAGIC,
               op0=ALU.mult, op1=ALU.add)
        self.I(eng, "tensor_scalar", R=W, W=W, out=tmp, in0=tmp, scalar1=-MAGIC, scalar2=float(-2 * np.pi),
               op0=ALU.add, op1=ALU.mult)
        self.I(eng, "tensor_tensor", R=list(R) + list(W), W=W, out=out, in0=x, in1=tmp, op=ALU.add)

    def mix_even(self, l):
        S, I, c = self.S, self.I, self.cst
        e = l // 2
        inp = self.inp
        R_scr = self.R("scratch_w")
        self.barrier()
        self.carve_reset()
        CV = self.carve
        ident = c[:, 0:128]
        Rc = self.R_c
        lre, lim, ldt = CV([16]), CV([16]), CV([16])
        t_a, t_b, t_c, t_d = CV([16]), CV([16]), CV([16]), CV([16])
        rmag, thr, zr, zi = CV([16]), CV([16]), CV([16]), CV([16])
        phi0, phit = CV([16]), CV([16])
        dsk, bglu, balp, gnorm = CV([4]), CV([4]), CV([4]), CV([4])
        bul = CV([16, 2, 128], BF16)
        cl = CV([16, 3, 128], BF16)
        wup = CV([256], BF16)
        wglu = CV([4, 512], BF16)
        walr = CV([8, 16], BF16)
        ycat = CV([8, TT], BF16)
        st = CV([4, 128])
        st_bf = CV([4, 128], BF16)
        zprev = CV([16, 2])
        mark = self.aoff
        Bre, Bim = CV([16, 16]), CV([16, 16])
        bbre, bbim, bbt = CV([16, 16]), CV([16, 16]), CV([16, 16])
        Cn = [CV([4, 64]), CV([4, 64])]
        Tin = [[CV([128]) for ri in range(2)] for q in range(4)]
        Tin2 = [CV([128]) for ri in range(2)]
        self.aoff = mark
        u_bf = CV([4, TT], BF16)
        cosT, sinT, tba, tbk = CV([TT]), CV([TT]), CV([TT]), CV([TT])
        zin = [CV([TT]), CV([TT])]
        zt = CV([TT])
        zz = [CV([TT]), CV([TT])]
        prods = [CV([TT], BF16) for i in range(4)]
        ysb = CV([TT])
        yg = CV([4, TT], BF16)
        end1 = self.aoff
        self.aoff = mark
        alr_bf = CV([TT], BF16)
        lsb = CV([TT])
        Scum = CV([TT])
        eq, ek = CV([TT]), CV([TT])
        eglast = CV([4, 8])
        q_dec, k_dec = CV([4, TT], BF16), CV([4, TT], BF16)
        k_upd = CV([4, TT])
        v_tm = CV([8, 512], BF16)
        sT = CV([256], BF16)
        kupd_tm = CV([256], BF16)
        o_sb = CV([4, TT])
        self.aoff = max(end1, self.aoff)
        silr = ycat[:, 4:8, :]
        ones128 = CV([128], BF16)
        negb = CV([4])
        Rn = self.R
        R_par = Rn("ev_par")

        def pd(out, in_, q="sp"):
            S.dma(q, (lambda e_, out=out, in_=in_: e_.dma_start(out=out, in_=in_, allow_slow_non_contiguous=True)),
                  semkey="evpar")
        pd(lre, inp["s5_lambda_re"][e].rearrange("(pr g2) p -> (g2 p) pr", g2=2))
        pd(lim, inp["s5_lambda_im"][e].rearrange("(pr g2) p -> (g2 p) pr", g2=2))
        ldt_src = inp["s5_log_dt"][e].rearrange("(pr g2) -> g2 pr", g2=2)
        for g2 in range(2):
            pd(ldt[64 * g2:64 * g2 + 64, :], ldt_src[g2:g2 + 1, :].partition_broadcast(64))
        pd(Bre, inp["s5_b_re"][e].rearrange("(pr g2) p j -> (g2 p) pr j", g2=2))
        pd(Bim, inp["s5_b_im"][e].rearrange("(pr g2) p j -> (g2 p) pr j", g2=2))
        pd(Cn[0], inp["s5_c_re"][e].rearrange("(ft g8) i p -> (g8 i) ft p", g8=8))
        pd(Cn[1], inp["s5_c_im"][e].rearrange("(ft g8) i p -> (g8 i) ft p", g8=8))
        pd(dsk, inp["s5_d"][e].rearrange("(ft g8) i -> (g8 i) ft", g8=8))
        pd(bglu, inp["s5_b_glu"][e].rearrange("(t p) -> p t", p=128))
        pd(balp[0:64, :], inp["gla_b_alpha"][e].rearrange("(h p) -> p h", p=64))
        pd(gnorm, inp["gla_norm"][e].rearrange("(t p) -> p t", p=128))
        pd(wup[0:16, :], inp["gla_w_alpha_up"][e], q="pool")
        pd(wglu, inp["s5_w_glu"][e].rearrange("(kt p) c -> p kt c", p=128), q="pool")
        pd(walr, inp["ev_w_in"][e][:, 2048:2064].rearrange("(kt p) c -> p kt c", p=128), q="pool")
        R_par.w = (("d", "evpar"), 16 * S.dcnt[("d", "evpar")])
        R_par.r = []

        Rt = Rn("ev_tiny")
        P = [R_par, Rc]
        I("act", "activation", R=P, W=[Rt], out=t_a, in_=ldt, func=AF.Exp)
        I("dve", "tensor_tensor", R=P + [Rt], W=[Rt], out=t_b, in0=lre, in1=t_a, op=ALU.mult)
        I("act", "activation", R=[Rt], W=[Rt], out=rmag, in_=t_b, func=AF.Exp)
        I("dve", "tensor_tensor", R=P + [Rt], W=[Rt], out=t_b, in0=lim, in1=t_a, op=ALU.mult)
        self.rr3("dve", thr, t_b, t_c, [Rt], [Rt])
        I("act", "activation", R=[Rt], W=[Rt], out=t_a, in_=thr, func=AF.Sin)
        I("act", "activation", R=[Rt], W=[Rt], out=t_b, in_=thr, func=AF.Abs)
        I("act", "activation", R=[Rt, Rc], W=[Rt], out=t_b, in_=t_b, func=AF.Sin, scale=-1.0, bias=c[:, 129:130])
        I("dve", "tensor_tensor", R=[Rt], W=[Rt], out=t_a, in0=t_a, in1=rmag, op=ALU.mult)
        I("dve", "tensor_tensor", R=[Rt], W=[Rt], out=t_b, in0=t_b, in1=rmag, op=ALU.mult)
        I("dve", "tensor_scalar", R=[Rt], W=[Rt], out=t_b, in0=t_b, scalar1=-1.0, scalar2=None, op0=ALU.add)
        I("dve", "tensor_tensor", R=P + [Rt], W=[Rt], out=t_c, in0=lre, in1=lre, op=ALU.mult)
        I("dve", "tensor_tensor", R=P + [Rt], W=[Rt], out=t_d, in0=lim, in1=lim, op=ALU.mult)
        I("dve", "tensor_tensor", R=[Rt], W=[Rt], out=t_c, in0=t_c, in1=t_d, op=ALU.add)
        I("dve", "reciprocal", R=[Rt], W=[Rt], out=t_c, in_=t_c)
        I("dve", "tensor_tensor", R=P + [Rt], W=[Rt], out=zr, in0=t_b, in1=lre, op=ALU.mult)
        I("dve", "tensor_tensor", R=P + [Rt], W=[Rt], out=t_d, in0=t_a, in1=lim, op=ALU.mult)
        I("dve", "tensor_tensor", R=[Rt], W=[Rt], out=zr, in0=zr, in1=t_d, op=ALU.add)
        I("dve", "tensor_tensor", R=[Rt], W=[Rt], out=zr, in0=zr, in1=t_c, op=ALU.mult)
        I("dve", "tensor_tensor", R=P + [Rt], W=[Rt], out=zi, in0=t_a, in1=lre, op=ALU.mult)
        I("dve", "tensor_tensor", R=P + [Rt], W=[Rt], out=t_d, in0=t_b, in1=lim, op=ALU.mult)
        I("dve", "tensor_tensor", R=[Rt], W=[Rt], out=zi, in0=zi, in1=t_d, op=ALU.subtract)
        I("dve", "tensor_tensor", R=[Rt], W=[Rt], out=zi, in0=zi, in1=t_c, op=ALU.mult)
        zr_b, zi_b = self.bc(zr, 16), self.bc(zi, 16)
        I("dve", "tensor_tensor", R=P + [Rt], W=[Rt], out=bbre, in0=Bre, in1=zr_b, op=ALU.mult)
        I("dve", "tensor_tensor", R=P + [Rt], W=[Rt], out=bbt, in0=Bim, in1=zi_b, op=ALU.mult)
        I("dve", "tensor_tensor", R=[Rt], W=[Rt], out=bbre, in0=bbre, in1=bbt, op=ALU.subtract)
        I("dve", "tensor_tensor", R=P + [Rt], W=[Rt], out=bbim, in0=Bim, in1=zr_b, op=ALU.mult)
        I("dve", "tensor_tensor", R=P + [Rt], W=[Rt], out=bbt, in0=Bre, in1=zi_b, op=ALU.mult)
        I("dve", "tensor_tensor", R=[Rt], W=[Rt], out=bbim, in0=bbim, in1=bbt, op=ALU.add)
        I("dve", "tensor_scalar", R=P, W=[Rt], out=negb[0:64, :], in0=balp[0:64, :], scalar1=-1.0, scalar2=None, op0=ALU.mult)
        I("dve", "memset", W=[Rn("ones128")], ap=ones128, constant=1.0 / 128)
        for q in range(4):
            for ri in range(2):
                I("pool", "memset", W=[Rn(f"Tin{q}{ri}")], ap=Tin[q][ri], constant=0.0)
        for pr in range(16):
            q = pr % 4
            for ri, bb in enumerate((bbre, bbim)):
                RT = Rn(f"Tin{q}{ri}")
                I("dve", "tensor_copy", R=[Rt], W=[RT], out=Tin[q][ri][0:64, 32 * q:32 * q + 16], in_=bb[0:64, pr, :])
                I("dve", "tensor_copy", R=[Rt], W=[RT], out=Tin[q][ri][64:128, 32 * q + 16:32 * q + 32], in_=bb[64:128, pr, :])
                pb, Rp = self.psum[ri], self.R_ps[ri]
                I("pe", "transpose", R=[RT, Rc], W=[Rp], out=pb[:, 0:128], in_=Tin[q][ri], identity=ident)
                I("act", "activation", R=[Rp], W=[Rn("bul")], out=bul[:, pr, ri, :], in_=pb[:, 0:128], func=AF.Copy)
        for pr in range(16):
            ft, q = pr // 4, pr % 4
            for ri in range(2):
                RT = Rn(f"Tin2{ri}")
                I("dve", "tensor_scalar", R=P, W=[RT], out=Tin2[ri][:, 0:64], in0=Cn[ri][:, ft, :],
                  scalar1=c[:, 136 + 2 * q:137 + 2 * q], scalar2=None, op0=ALU.mult)
                I("dve", "tensor_scalar", R=P, W=[RT], out=Tin2[ri][:, 64:128], in0=Cn[ri][:, ft, :],
                  scalar1=c[:, 137 + 2 * q:138 + 2 * q], scalar2=None, op0=ALU.mult)
                pb, Rp = self.psum[2 + ri], self.R_ps[2 + ri]
                I("pe", "transpose", R=[RT, Rc], W=[Rp], out=pb[:, 0:128], in_=Tin2[ri], identity=ident)
                if ri == 0:
                    I("act", "activation", R=[Rp], W=[Rn("cl")], out=cl[:, pr, 0, :], in_=pb[:, 0:128], func=AF.Copy)
                    I("act", "activation", R=[Rp], W=[Rn("cl")], out=cl[:, pr, 1, :], in_=pb[:, 0:128], func=AF.Copy, scale=-1.0)
                else:
                    I("act", "activation", R=[Rp], W=[Rn("cl")], out=cl[:, pr, 2, :], in_=pb[:, 0:128], func=AF.Copy, scale=-1.0)
        I("pool", "memset", W=[Rn("zprev")], ap=zprev, constant=0.0)
        I("pool", "memset", W=[Rn("st")], ap=st, constant=0.0)
        I("pool", "memset", W=[Rn("st_bf")], ap=st_bf, constant=0.0)

        sc_ein, sc_eout = self.sc_ein, self.sc_eout
        R_hn = Rn("hn")
        maskT = c[0:64, 256:320]
        iota = c[:, 512:1024]
        cmask = c[:, 1024:1536]

        def proj(blk, psi):
            w, Rw = self.load_w(sc_ein[e, blk], R_scr)
            for k in range(8):
                I("pe", "matmul", R=[Rw, R_hn], W=[self.R_ps[psi]], out=self.psum[psi][:, :], lhsT=w[:, k, :],
                  rhs=self.hn[:, k, :], start=(k == 0), stop=(k == 7))
            return self.psum[psi], self.R_ps[psi]

        import os
        stage = int(os.environ.get('DBG_STAGE', '9'))
        self.barrier()
        for tt in range(NTT if stage > 0 else 0):
            t0 = tt * TT
            self.prenorm(tt, self.lnw["ln_mix_pre"], l)
            for ft in range(4):
                pb, Rp = proj(ft, ft % 2)
                I("act", "activation", R=[Rp], W=[Rn("u_bf")], out=u_bf[:, ft, :], in_=pb[:, :], func=AF.Copy)
            I("dve", "tensor_scalar", R=[Rt], W=[Rn("phi")], out=phit, in0=thr, scalar1=float(t0), scalar2=None, op0=ALU.mult)
            self.rr3("dve", phi0, phit, t_d, [Rn("phi")], [Rn("phi")])
            for ft in range(4):
                for q in range(4):
                    pr = 4 * ft + q
                    pre, Rpre = self.psum[2], self.R_ps[2]
                    pim, Rpim = self.psum[3], self.R_ps[3]
                    I("pe", "matmul", R=[Rn("bul"), Rn("u_bf")], W=[Rpre], out=pre[:, :], lhsT=bul[:, pr, 0, :], rhs=u_bf[:, ft, :],
                      start=True, stop=True)
                    I("pe", "matmul", R=[Rn("bul"), Rn("u_bf")], W=[Rpim], out=pim[:, :], lhsT=bul[:, pr, 1, :], rhs=u_bf[:, ft, :],
                      start=True, stop=True)
                    Rtab = Rn("tab")
                    I("pool", "tensor_scalar", R=[Rt, Rn("phi"), Rc], W=[Rn("tba")], out=tba, in0=iota, scalar1=thr[:, pr:pr + 1],
                      scalar2=phi0[:, pr:pr + 1], op0=ALU.mult, op1=ALU.add)
                    self.rr3("pool", tba, tba, tbk, [Rn("tba")], [Rn("tba"), Rn("tbk")])
                    I("act", "activation", R=[Rn("tba")], W=[Rn("sinT")], out=sinT, in_=tba, func=AF.Sin)
                    I("act", "activation", R=[Rn("tba")], W=[Rn("tbk")], out=tbk, in_=tba, func=AF.Abs)
                    I("act", "activation", R=[Rn("tbk"), Rc], W=[Rn("cosT")], out=cosT, in_=tbk, func=AF.Sin, scale=-1.0,
                      bias=c[:, 129:130])
                    I("dve", "tensor_tensor", R=[Rn("cosT"), Rpre], W=[Rn("zin0")], out=zin[0], in0=cosT, in1=pre[:, :], op=ALU.mult)
                    I("dve", "tensor_tensor", R=[Rn("sinT"), Rpim], W=[Rn("zt")], out=zt, in0=sinT, in1=pim[:, :], op=ALU.mult)
                    I("dve", "tensor_tensor", R=[Rn("zt")], W=[Rn("zin0")], out=zin[0], in0=zin[0], in1=zt, op=ALU.add)
                    I("dve", "tensor_tensor", R=[Rn("cosT"), Rpim], W=[Rn("zin1")], out=zin[1], in0=cosT, in1=pim[:, :], op=ALU.mult)
                    I("dve", "tensor_tensor", R=[Rn("sinT"), Rpre], W=[Rn("zt")], out=zt, in0=sinT, in1=pre[:, :], op=ALU.mult)
                    I("dve", "tensor_tensor", R=[Rn("zt")], W=[Rn("zin1")], out=zin[1], in0=zin[1], in1=zt, op=ALU.subtract)
                    for ri in range(2):
                        I("dve", "tensor_tensor_scan", R=[Rn(f"zin{ri}"), Rt, Rn("zprev")], W=[Rn(f"zz{ri}")], out=zz[ri],
                          data0=rmag[:, pr:pr + 1].to_broadcast([128, TT]),
                          data1=zin[ri], initial=zprev[:, pr, ri:ri + 1], op0=ALU.mult, op1=ALU.add)
                        I("act", "activation", R=[Rn(f"zz{ri}")], W=[Rn("zprev")], out=zprev[:, pr, ri:ri + 1],
                          in_=zz[ri][:, TT - 1:TT], func=AF.Copy)
                    I("pool", "tensor_tensor", R=[Rn("cosT"), Rn("zz0")], W=[Rn("pr0")], out=prods[0], in0=cosT, in1=zz[0], op=ALU.mult)
                    I("pool", "tensor_tensor", R=[Rn("sinT"), Rn("zz1")], W=[Rn("pr1")], out=prods[1], in0=sinT, in1=zz[1], op=ALU.mult)
                    I("pool", "tensor_tensor", R=[Rn("sinT"), Rn("zz0")], W=[Rn("pr2")], out=prods[2], in0=sinT, in1=zz[0], op=ALU.mult)
                    I("pool", "tensor_tensor", R=[Rn("cosT"), Rn("zz1")], W=[Rn("pr3")], out=prods[3], in0=cosT, in1=zz[1], op=ALU.mult)
                    py, Rpy = self.psum[4], self.R_ps[4]
                    for i, var in enumerate((0, 1, 2, 2)):
                        I("pe", "matmul", R=[Rn("cl"), Rn(f"pr{i}")], W=[Rpy], out=py[:, :], lhsT=cl[:, pr, var, :], rhs=prods[i],
                          start=(q == 0 and i == 0), stop=(q == 3 and i == 3))
                py, Rpy = self.psum[4], self.R_ps[4]
                I("dve", "scalar_tensor_tensor", R=[Rn("u_bf"), P[0], Rpy], W=[Rn("ysb")], out=ysb, in0=u_bf[:, ft, :],
                  scalar=dsk[:, ft:ft + 1], in1=py[:, :], op0=ALU.mult, op1=ALU.add)
                I("act", "activation", R=[Rn("ysb")], W=[Rn("yg")], out=yg[:, ft, :], in_=ysb, func=AF.Gelu_apprx_tanh)
            for ct in range(4):
                pb, Rp = self.psum[5], self.R_ps[5]
                for k in range(4):
                    I("pe", "matmul", R=[R_par, Rn("yg")], W=[Rp], out=pb[:, :], lhsT=wglu[:, k, ct * 128:(ct + 1) * 128],
                      rhs=yg[:, k, :], start=(k == 0), stop=(k == 3))
                I("act", "activation", R=[Rp, R_par], W=[Rn("ysb")], out=ysb, in_=pb[:, :], func=AF.Sigmoid, bias=bglu[:, ct:ct + 1])
                I("pool", "tensor_tensor", R=[Rn("ysb"), Rn("yg")], W=[Rn("ycat")], out=ycat[:, ct, :], in0=yg[:, ct, :], in1=ysb,
                  op=ALU.mult)
            if stage < 2:
                continue
            self.barrier()
            pb, Rp = self.psum[0], self.R_ps[0]
            for k in range(8):
                I("pe", "matmul", R=[R_par, R_hn], W=[Rp], out=pb[0:16, :], lhsT=walr[:, k, :], rhs=self.hn[:, k, :],
                  start=(k == 0), stop=(k == 7))
            I("act", "activation", R=[Rp], W=[Rn("alr")], out=alr_bf[0:16, :], in_=pb[0:16, :], func=AF.Copy)
            for t2 in range(2):
                wq, Rwq = self.load_w(sc_ein[e, 4 + t2], R_scr)
                wk, Rwk = self.load_w(sc_ein[e, 6 + t2], R_scr)
                for hh in range(2):
                    h = 2 * t2 + hh
                    pb, Rp = self.psum[1], self.R_ps[1]
                    I("pe", "matmul", R=[R_par, Rn("alr")], W=[Rp], out=pb[0:64, :], lhsT=wup[0:16, h * 64:(h + 1) * 64],
                      rhs=alr_bf[0:16, :], start=True, stop=True)
                    I("act", "activation", R=[Rp, Rt], W=[Rn("lsb")], out=lsb[0:64, :], in_=pb[0:64, :], func=AF.Exp, scale=-1.0,
                      bias=negb[0:64, h:h + 1])
                    I("act", "activation", R=[Rn("lsb"), Rc], W=[Rn("lsb")], out=lsb[0:64, :], in_=lsb[0:64, :], func=AF.Ln,
                      bias=c[0:64, 130:131])
                    I("dve", "tensor_tensor_scan", R=[Rn("lsb"), Rc], W=[Rn("Scum")], out=Scum[0:64, :], data0=cmask[0:64, :],
                      data1=lsb[0:64, :], initial=0.0, op0=ALU.mult, op1=ALU.add)
                    I("act", "activation", R=[Rn("Scum")], W=[Rn("eq")], out=eq[0:64, :], in_=Scum[0:64, :], func=AF.Exp,
                      scale=-1.0 / 16)
                    I("act", "activation", R=[Rn("Scum")], W=[Rn("ek")], out=ek[0:64, :], in_=Scum[0:64, :], func=AF.Exp,
                      scale=1.0 / 16)
                    I("act", "activation", R=[Rn("Scum")], W=[Rn("eglast")], out=eglast[0:64, h, :],
                      in_=Scum[0:64, :].rearrange("p (c t) -> p c t", t=64)[:, :, 63], func=AF.Exp, scale=-1.0 / 16)
                    pq, Rpq = self.psum[2], self.R_ps[2]
                    for k in range(8):
                        I("pe", "matmul", R=[Rwq, R_hn], W=[Rpq], out=pq[0:64, :], lhsT=wq[:, k, hh * 64:(hh + 1) * 64],
                          rhs=self.hn[:, k, :], start=(k == 0), stop=(k == 7))
                    I("dve", "scalar_tensor_tensor", R=[Rpq, Rn("eq")], W=[Rn("q_dec")], out=q_dec[0:64, h, :], in0=pq[0:64, :],
                      scalar=0.125, in1=eq[0:64, :], op0=ALU.mult, op1=ALU.mult)
                    pk, Rpk = self.psum[3], self.R_ps[3]
                    for k in range(8):
                        I("pe", "matmul", R=[Rwk, R_hn], W=[Rpk], out=pk[0:64, :], lhsT=wk[:, k, hh * 64:(hh + 1) * 64],
                          rhs=self.hn[:, k, :], start=(k == 0), stop=(k == 7))
                    I("dve", "tensor_tensor", R=[Rpk, Rn("ek")], W=[Rn("k_dec")], out=k_dec[0:64, h, :], in0=pk[0:64, :],
                      in1=ek[0:64, :], op=ALU.mult)
                    I("pool", "tensor_tensor", R=[Rn("k_dec"), Rn("eglast")], W=[Rn("k_upd")],
                      out=k_upd[0:64, h, :].rearrange("p (c t) -> p c t", t=64),
                      in0=k_dec[0:64, h, :].rearrange("p (c t) -> p c t", t=64), in1=self.bc(eglast[0:64, h, :], 64), op=ALU.mult)
            for h in range(4):
                pb, Rp = proj(12 + h, h % 2)
                I("act", "activation", R=[Rp], W=[Rn("silr")], out=silr[:, h, :], in_=pb[:, :], func=AF.Silu)
            wv = [self.load_w(sc_ein[e, 8 + b], R_scr) for b in range(4)]
            for ch in range(8):
                pb, Rp = self.psum[ch % 2], self.R_ps[ch % 2]
                for b in range(4):
                    for k in range(8):
                        I("pe", "matmul", R=[wv[b][1], R_hn], W=[Rp], out=pb[0:64, b * 128:(b + 1) * 128],
                          lhsT=self.hn[:, k, ch * 64:(ch + 1) * 64], rhs=wv[b][0][:, k, :], start=(k == 0), stop=(k == 7))
                I("act", "activation", R=[Rp], W=[Rn("v_tm")], out=v_tm[0:64, ch, :], in_=pb[0:64, :], func=AF.Copy)
            if stage < 3:
                continue
            for ch in range(8):
                cs = slice(ch * 64, (ch + 1) * 64)
                ps_s, Rs_ = self.psum[2], self.R_ps[2]
                for h in range(4):
                    I("pe", "matmul", R=[Rn("k_dec"), Rn("q_dec")], W=[Rs_], out=ps_s[0:64, h * 64:(h + 1) * 64],
                      lhsT=k_dec[0:64, h, cs], rhs=q_dec[0:64, h, cs], start=True, stop=True)
                I("dve", "tensor_tensor", R=[Rs_, Rc], W=[Rn("sT")], out=sT[0:64, :].rearrange("p (h i) -> p h i", h=4),
                  in0=ps_s[0:64, 0:256].rearrange("p (h i) -> p h i", h=4), in1=self.bc(maskT, 4, pos=0), op=ALU.mult)
                ps_t, Rt_ = self.psum[3], self.R_ps[3]
                for h in range(4):
                    I("pe", "transpose", R=[Rn("k_upd"), Rc], W=[Rt_], out=ps_t[0:64, h * 64:(h + 1) * 64],
                      in_=k_upd[0:64, h, cs], identity=c[0:64, 0:64])
                I("act", "activation", R=[Rt_], W=[Rn("kupd_tm")], out=kupd_tm[0:64, :], in_=ps_t[0:64, 0:256], func=AF.Copy)
                ps_o, Ro_ = self.psum[5], self.R_ps[5]
                for h in range(4):
                    I("pe", "matmul", R=[Rn("v_tm"), Rn("sT")], W=[Ro_], out=ps_o[:, h * 64:(h + 1) * 64],
                      lhsT=v_tm[0:64, ch, h * 128:(h + 1) * 128], rhs=sT[0:64, h * 64:(h + 1) * 64], start=True, stop=False)
                    I("pe", "matmul", R=[Rn("st_bf"), Rn("q_dec")], W=[Ro_], out=ps_o[:, h * 64:(h + 1) * 64],
                      lhsT=st_bf[0:64, h, :], rhs=q_dec[0:64, h, cs], start=False, stop=True)
                I("act", "activation", R=[Ro_], W=[Rn("o_sb")], out=o_sb[:, :, cs],
                  in_=ps_o[:, 0:256].rearrange("p (h i) -> p h i", h=4), func=AF.Copy)
                ps_d, Rd_ = self.psum[6], self.R_ps[6]
                for h in range(4):
                    I("pe", "matmul", R=[Rn("kupd_tm"), Rn("v_tm")], W=[Rd_], out=ps_d[0:64, h * 128:(h + 1) * 128],
                      lhsT=kupd_tm[0:64, h * 64:(h + 1) * 64], rhs=v_tm[0:64, ch, h * 128:(h + 1) * 128], start=True, stop=True)
                for h in range(4):
                    I("dve", "scalar_tensor_tensor", R=[Rd_, Rn("eglast")], W=[Rn("st")], out=st[0:64, h, :],
                      in0=st[0:64, h, :], scalar=eglast[0:64, h, ch:ch + 1], in1=ps_d[0:64, h * 128:(h + 1) * 128],
                      op0=ALU.mult, op1=ALU.add)
                I("act", "activation", R=[Rn("st")], W=[Rn("st_bf")], out=st_bf[0:64, :, :], in_=st[0:64, :, :], func=AF.Copy)
            if stage < 4:
                continue
            for h in range(4):
                self.rms_stats([o_sb[:, h, :]], [Rn("o_sb")], ones128, 1, ones_res=Rn("ones128"))
                Rtm = Rn("tmpf0")
                I("dve", "scalar_tensor_tensor", R=[Rn("o_sb"), Rn("rstd"), R_par], W=[Rtm], out=self.tmpf[0][:], in0=o_sb[:, h, :],
                  scalar=gnorm[:, h:h + 1], in1=self.rstd[:], op0=ALU.mult, op1=ALU.mult)
                I("pool", "tensor_tensor", R=[Rtm, Rn("silr")], W=[Rn("silr")], out=ycat[:, 4 + h, :], in0=self.tmpf[0][:],
                  in1=silr[:, h, :], op=ALU.mult)
            R_f = Rn("fbuf")
            for ob in range(8):
                w, Rw = self.load_w(sc_eout[e, ob], R_scr)
                pb, Rp = self.psum[ob % 2], self.R_ps[ob % 2]
                for k in range(8):
                    I("pe", "matmul", R=[Rw, Rn("ycat"), Rn("silr")], W=[Rp], out=pb[:, :], lhsT=w[:, k, :], rhs=ycat[:, k, :],
                      start=(k == 0), stop=(k == 7))
                I("act", "activation", R=[Rp], W=[R_f], out=self.fbuf[:, ob, :], in_=pb[:, :], func=AF.Copy)
            self.epilogue(tt, self.lnw["ln_mix_post"], l)
            self.barrier()


    def prologue_odd(self, used_layers):
        S, inp = self.S, self.inp
        self.sc_oin = self.dscratch("sc_oin", [2, 32, 128, 8, 128])
        self.sc_oout = self.dscratch("sc_oout", [2, 8, 2, 128, 8, 128])
        for o in sorted(set(l // 2 for l in used_layers if l % 2 == 1)):
            for fo in range(32):
                src = inp["od_w_in"][o, :, fo * 128:(fo + 1) * 128].rearrange("(kt p) c -> p kt c", p=128)
                S.dma("pool", (lambda e, dst=self.sc_oin[o, fo], src=src: e.dma_start(out=dst, in_=src)), semkey="prolog")
            for ob in range(8):
                for hf in range(2):
                    src = inp["od_w_out"][o, hf * 1024:(hf + 1) * 1024, ob * 128:(ob + 1) * 128].rearrange(
                        "(kt p) c -> p kt c", p=128)
                    S.dma("pool", (lambda e, dst=self.sc_oout[o, ob, hf], src=src: e.dma_start(out=dst, in_=src)),
                          semkey="prolog")

    def mix_odd(self, l):
        S, I, c = self.S, self.I, self.cst
        o = l // 2
        inp = self.inp
        Rn = self.R
        Rc = self.R_c
        R_scr = Rn("scratch_w")
        self.barrier()
        self.carve_reset()
        CV = self.carve
        TM = 64
        DHS = float(512 ** -0.5)
        ident = c[:, 0:128]
        tri = c[0:64, 320:384]
        maskT = c[0:64, 256:320]
        BDM = c[:, 384:512]
        Cst = CV([16, 512])
        Cbf = CV([4, 512], BF16)
        nst, nbf = CV([16]), CV([16], BF16)
        mrow = CV([1])
        hist = CV([16, 3])
        Wq, Wk, Wv = CV([16, 128], BF16), CV([16, 128], BF16), CV([16, 128], BF16)
        wgt = CV([3, 16, 8], BF16)
        cw, cb, ng, sk = CV([16, 4]), CV([16]), CV([16]), CV([16])
        bg_bc = CV([8])
        onesb = CV([8], BF16)
        wraw = CV([3, 16, 4])
        xmT, xcT, qT, kT, vT, ogT = [CV([16, TM], BF16) for _ in range(6)]
        cvb = CV([TM])
        k_tm, v_tm = CV([2048], BF16), CV([2048], BF16)
        g_tm, lpos, a_tm = CV([8]), CV([4]), CV([4])
        a_row, F_row, M_row, wi_row, em_row, wu_row = [CV([64]) for _ in range(6)]
        negM, diagd, dec_bc, pk, sclk = CV([1]), CV([4]), CV([4]), CV([12]), CV([4])
        E, sTf, sT = CV([256]), CV([256]), CV([256], BF16)
        dint, den, nden, rden, wr, ss, rs = [CV([4]) for _ in range(7)]
        t1, hh = CV([512]), CV([512])
        u1, u2 = CV([4, 64]), CV([4, 64])
        R_par = Rn("od_par")

        def pd(out, in_, q="sp"):
            S.dma(q, (lambda e_, out=out, in_=in_: e_.dma_start(out=out, in_=in_, allow_slow_non_contiguous=True)),
                  semkey="odpar")
        for si, nm in enumerate(("ml_w_q", "ml_w_k", "ml_w_v")):
            pd(wraw[:, si, :, :], inp[nm][o].rearrange("(ft nl) c d -> (nl c) ft d", nl=32))
        for si in range(3):
            pd(wgt[:, si, :, :], inp["ml_w_gate"][o, si].rearrange("(ft p) g -> p ft g", p=128), q="pool")
        for k in range(4):
            pd(cw[:, :, k], inp["ml_conv_w"][o, k].rearrange("(ft p) -> p ft", p=128))
        pd(cb, inp["ml_conv_b"][o].rearrange("(ft p) -> p ft", p=128))
        pd(ng, inp["ml_norm"][o].rearrange("(ft p) -> p ft", p=128))
        pd(sk, inp["ml_skip"][o].rearrange("(ft p) -> p ft", p=128))
        pd(bg_bc[0:64, :], inp["ml_b_gate"][o:o + 1, :].partition_broadcast(64))
        R_par.w = (("d", "odpar"), 16 * S.dcnt[("d", "odpar")])
        R_par.r = []
        P = [R_par, Rc]
        for si, W in enumerate((Wq, Wk, Wv)):
            for ft in range(16):
                I("dve" if ft % 2 == 0 else "pool", "tensor_tensor", R=P, W=[Rn("Wbd")],
                  out=W[:, ft, :].rearrange("p (n d) -> p n d", d=4), in0=BDM.rearrange("p (n d) -> p n d", d=4),
                  in1=self.bc(wraw[:, si, ft, :], 32, pos=0), op=ALU.mult)
        I("pool", "memset", W=[Rn("Cst")], ap=Cst, constant=0.0)
        I("pool", "memset", W=[Rn("nst")], ap=nst, constant=0.0)
        I("pool", "memset", W=[Rn("nbf")], ap=nbf, constant=0.0)
        I("pool", "memset", W=[Rn("mrow")], ap=mrow, constant=0.0)
        I("pool", "memset", W=[Rn("hist")], ap=hist, constant=0.0)
        I("pool", "memset", W=[Rn("onesb")], ap=onesb, constant=1.0)
        self.barrier()
        R_hn = Rn("hn")
        sc_oin, sc_oout = self.sc_oin, self.sc_oout
        import os
        stage = int(os.environ.get('DBG_STAGE', '9'))
        nchunks = int(os.environ.get('DBG_NCH', str(L // TM)))

        for ch in range(nchunks):
            t0 = ch * TM
            self.prenorm(None, self.lnw["ln_mix_pre"], l, t0=t0, n=TM)
            for fo in range(32):
                w, Rw = self.load_w(sc_oin[o, fo], R_scr)
                pb, Rp = self.psum[fo % 2], self.R_ps[fo % 2]
                for k in range(8):
                    I("pe", "matmul", R=[Rw, R_hn], W=[Rp], out=pb[:, 0:TM], lhsT=w[:, k, :], rhs=self.hn[:, k, 0:TM],
                      start=(k == 0), stop=(k == 7))
                if fo < 16:
                    ft = fo
                    I("act", "activation", R=[Rp], W=[Rn("xmT")], out=xmT[:, ft, :], in_=pb[:, 0:TM], func=AF.Copy)
                    I("act", "activation", R=[Rp] + P, W=[Rn("cvb")], out=cvb, in_=pb[:, 0:TM], func=AF.Identity,
                      bias=cb[:, ft:ft + 1], scale=cw[:, ft, 3:4])
                    for sh in (1, 2, 3):
                        I("dve", "scalar_tensor_tensor", R=[Rp] + P, W=[Rn("cvb")], out=cvb[:, sh:TM], in0=pb[:, 0:TM - sh],
                          scalar=cw[:, ft, 3 - sh:4 - sh], in1=cvb[:, sh:TM], op0=ALU.mult, op1=ALU.add)
                    for sh in (1, 2, 3):
                        I("dve", "scalar_tensor_tensor", R=[Rn("hist")] + P, W=[Rn("cvb")], out=cvb[:, 0:sh],
                          in0=hist[:, ft, 3 - sh:3], scalar=cw[:, ft, 3 - sh:4 - sh], in1=cvb[:, 0:sh], op0=ALU.mult, op1=ALU.add)
                    I("act", "activation", R=[Rp, Rn("cvb")], W=[Rn("hist")], out=hist[:, ft, :], in_=pb[:, TM - 3:TM], func=AF.Copy)
                    I("act", "activation", R=[Rn("cvb")], W=[Rn("xcT")], out=xcT[:, ft, :], in_=cvb, func=AF.Silu)
                else:
                    ft = fo - 16
                    I("act", "activation", R=[Rp], W=[Rn("ogT")], out=ogT[:, ft, :], in_=pb[:, 0:TM], func=AF.Sigmoid)
            if stage < 2:
                continue
            for W, src, dst, nm in ((Wq, xcT, qT, "qT"), (Wk, xcT, kT, "kT"), (Wv, xmT, vT, "vT")):
                for g4 in range(4):
                    pb, Rp = self.psum[g4 % 2], self.R_ps[g4 % 2]
                    for j in range(4):
                        ft = 4 * g4 + j
                        I("pe", "matmul", R=[Rn("Wbd"), Rn("xcT"), Rn("xmT")], W=[Rp], out=pb[:, j * TM:(j + 1) * TM],
                          lhsT=W[:, ft, :], rhs=src[:, ft, :], start=True, stop=True)
                    I("act" if g4 % 2 == 0 else "dve", "activation" if g4 % 2 == 0 else "tensor_copy", R=[Rp], W=[Rn(nm)],
                      **(dict(out=dst[:, 4 * g4:4 * g4 + 4, :], in_=pb[:, 0:4 * TM].rearrange("p (j t) -> p j t", j=4), func=AF.Copy)
                         if g4 % 2 == 0 else
                         dict(out=dst[:, 4 * g4:4 * g4 + 4, :], in_=pb[:, 0:4 * TM].rearrange("p (j t) -> p j t", j=4))))
            pg, Rpg = self.psum[3], self.R_ps[3]
            cnt = 0
            for si, src, nm in ((0, qT, "qT"), (1, kT, "kT"), (2, vT, "vT")):
                for ft in range(16):
                    I("pe", "matmul", R=[Rn(nm)] + P, W=[Rpg], out=pg[0:64, 0:8], lhsT=src[:, ft, :], rhs=wgt[:, si, ft, :],
                      start=(cnt == 0), stop=(cnt == 47))
                    cnt += 1
            Rg = Rn("gsm")
            I("dve", "tensor_tensor", R=[Rpg] + P, W=[Rg], out=g_tm[0:64, :], in0=pg[0:64, 0:8], in1=bg_bc[0:64, :], op=ALU.add)
            I("act", "activation", R=[Rg], W=[Rn("lpos")], out=lpos[0:64, :], in_=g_tm[0:64, 4:8], func=AF.Exp, scale=-1.0)
            I("act", "activation", R=[Rn("lpos"), Rc], W=[Rn("lpos")], out=lpos[0:64, :], in_=lpos[0:64, :], func=AF.Ln,
              bias=c[0:64, 130:131])
            I("pe", "matmul", R=[Rn("lpos"), Rc], W=[Rpg], out=pg[0:64, 16:20], lhsT=tri, rhs=lpos[0:64, :], start=True, stop=True)
            I("dve", "tensor_tensor", R=[Rpg, Rg], W=[Rn("a_tm")], out=a_tm[0:64, :], in0=pg[0:64, 16:20], in1=g_tm[0:64, 0:4],
              op=ALU.add)
            I("pe", "matmul", R=[Rn("a_tm"), Rc], W=[Rpg], out=pg[0:4, 32:96], lhsT=a_tm[0:64, :], rhs=ident[0:64, 0:64],
              start=True, stop=True)
            I("pe", "matmul", R=[Rn("lpos"), Rc], W=[Rpg], out=pg[0:4, 96:160], lhsT=lpos[0:64, :], rhs=tri, start=True, stop=True)
            Rrow = Rn("rows")
            I("act", "activation", R=[Rpg], W=[Rrow], out=a_row[0:4, :], in_=pg[0:4, 32:96], func=AF.Copy)
            I("act", "activation", R=[Rpg], W=[Rrow], out=F_row[0:4, :], in_=pg[0:4, 96:160], func=AF.Copy)
            I("dve", "tensor_tensor_scan", R=[Rrow, Rn("mrow"), Rc], W=[Rn("M_row")], out=M_row[0:4, :], data0=c[0:4, 1792:1856],
              data1=a_row[0:4, :], initial=mrow[0:4, 0:1], op0=ALU.mult, op1=ALU.max)
            I("act", "activation", R=[Rn("M_row"), Rn("mrow")], W=[Rn("wi_row")], out=wi_row[0:4, :], in_=M_row[0:4, :], func=AF.Exp,
              scale=-1.0, bias=mrow[0:4, 0:1])
            I("dve", "tensor_tensor", R=[Rrow, Rn("M_row")], W=[Rn("em_row")], out=em_row[0:4, :], in0=F_row[0:4, :], in1=M_row[0:4, :],
              op=ALU.subtract)
            I("act", "activation", R=[Rn("em_row")], W=[Rn("em_row")], out=em_row[0:4, :], in_=em_row[0:4, :], func=AF.Exp)
            I("dve", "tensor_scalar", R=[Rn("M_row")], W=[Rn("negM")], out=negM[0:4, :], in0=M_row[0:4, 63:64], scalar1=-1.0,
              scalar2=None, op0=ALU.mult)
            I("act", "activation", R=[Rrow, Rn("negM")], W=[Rn("wu_row")], out=wu_row[0:4, :], in_=a_row[0:4, :], func=AF.Exp,
              bias=negM[0:4, 0:1])
            I("dve", "tensor_scalar", R=[Rn("wi_row"), Rc], W=[Rn("diagd")], out=diagd[0:4, :], in0=ident[0:4, 0:4],
              scalar1=wi_row[0:4, 63:64], scalar2=None, op0=ALU.mult)
            I("dve", "tensor_tensor", R=[Rn("M_row"), Rrow, Rn("wi_row")], W=[Rn("mrow")], out=mrow[0:4, 0:1], in0=M_row[0:4, 63:64],
              in1=F_row[0:4, 63:64], op=ALU.subtract)
            for qi, (row, nm) in enumerate(((wi_row, "wi_row"), (em_row, "em_row"), (wu_row, "wu_row"))):
                I("pe", "matmul", R=[Rn(nm), Rc], W=[Rpg], out=pg[0:64, 160 + 4 * qi:164 + 4 * qi], lhsT=row[0:4, :],
                  rhs=ident[0:4, 0:4], start=True, stop=True)
            I("pe", "matmul", R=[Rn("diagd"), Rc], W=[Rpg], out=pg[:, 176:180], lhsT=c[0:4, 1856:1984], rhs=diagd[0:4, :],
              start=True, stop=True)
            I("act", "activation", R=[Rpg], W=[Rn("pk")], out=pk[0:64, :], in_=pg[0:64, 160:172], func=AF.Copy)
            I("act", "activation", R=[Rpg], W=[Rn("dec_bc")], out=dec_bc, in_=pg[:, 176:180], func=AF.Copy)
            I("dve", "tensor_scalar", R=[Rn("pk")], W=[Rn("sclk")], out=sclk[0:64, :], in0=pk[0:64, 8:12], scalar1=DHS, scalar2=None,
              op0=ALU.mult)
            ps_s, Rs_ = self.psum[2], self.R_ps[2]
            for h in range(4):
                I("pe", "matmul", R=[Rn("M_row"), Rc], W=[Rs_], out=ps_s[0:64, 256 + h * 64:256 + (h + 1) * 64],
                  lhsT=c[0:4, 1536 + h * 64:1536 + (h + 1) * 64], rhs=M_row[0:4, :], start=True, stop=True)
            if stage < 3:
                continue
            for h in range(4):
                pb, Rp = self.psum[h % 2], self.R_ps[h % 2]
                for j in range(4):
                    ft = 4 * h + j
                    I("pe", "matmul", R=[Rn("xcT"), Rn("Wbd")], W=[Rp], out=pb[0:64, j * 128:(j + 1) * 128], lhsT=xcT[:, ft, :],
                      rhs=Wk[:, ft, :], start=True, stop=True)
                I("act", "activation", R=[Rp, Rn("sclk")], W=[Rn("k_tm")], out=k_tm[0:64, h * 512:(h + 1) * 512], in_=pb[0:64, :],
                  func=AF.Copy, scale=sclk[0:64, h:h + 1])
            for h in range(4):
                pb, Rp = self.psum[h % 2], self.R_ps[h % 2]
                for j in range(4):
                    ft = 4 * h + j
                    I("pe", "matmul", R=[Rn("xmT"), Rn("Wbd")], W=[Rp], out=pb[0:64, j * 128:(j + 1) * 128], lhsT=xmT[:, ft, :],
                      rhs=Wv[:, ft, :], start=True, stop=True)
                I("dve", "tensor_copy", R=[Rp], W=[Rn("v_tm")], out=v_tm[0:64, h * 512:(h + 1) * 512], in_=pb[0:64, :])
            for h in range(4):
                for dt in range(4):
                    I("pe", "matmul", R=[Rn("kT"), Rn("qT")], W=[Rs_], out=ps_s[0:64, h * 64:(h + 1) * 64], lhsT=kT[:, 4 * h + dt, :],
                      rhs=qT[:, 4 * h + dt, :], start=(dt == 0), stop=(dt == 3))
            for h in range(4):
                I("act", "activation", R=[Rs_, Rn("a_tm")], W=[Rn("E")], out=E[0:64, h * 64:(h + 1) * 64],
                  in_=ps_s[0:64, 256 + h * 64:256 + (h + 1) * 64], func=AF.Exp, scale=-1.0, bias=a_tm[0:64, h:h + 1])
            I("dve", "scalar_tensor_tensor", R=[Rs_, Rn("E")], W=[Rn("sTf")], out=sTf[0:64, :], in0=ps_s[0:64, 0:256], scalar=DHS,
              in1=E[0:64, :], op0=ALU.mult, op1=ALU.mult)
            I("pool", "tensor_tensor", R=[Rn("sTf"), Rc], W=[Rn("sT")], out=sT[0:64, :].rearrange("p (h i) -> p h i", h=4),
              in0=sTf[0:64, :].rearrange("p (h i) -> p h i", h=4), in1=self.bc(maskT, 4, pos=0), op=ALU.mult)
            if stage < 4:
                continue
            psm, Rsm = self.psum[3], self.R_ps[3]
            for h in range(4):
                for dt in range(4):
                    I("act" if dt % 2 == 0 else "pool", "activation" if dt % 2 == 0 else "tensor_copy", R=[Rn("Cst")], W=[Rn("Cbf")],
                      **(dict(out=Cbf[:, dt, :], in_=Cst[:, 4 * h + dt, :], func=AF.Copy) if dt % 2 == 0 else
                         dict(out=Cbf[:, dt, :], in_=Cst[:, 4 * h + dt, :])))
                p_i, Rp_i = self.psum[4], self.R_ps[4]
                p_e, Rp_e = self.psum[5], self.R_ps[5]
                I("pe", "matmul", R=[Rn("sT"), Rn("v_tm")], W=[Rp_i], out=p_i[0:64, :], lhsT=sT[0:64, h * 64:(h + 1) * 64],
                  rhs=v_tm[0:64, h * 512:(h + 1) * 512], start=True, stop=True)
                I("pe", "matmul", R=[Rn("sT"), Rn("onesb")], W=[Rsm], out=psm[0:64, 192 + h:193 + h], lhsT=sT[0:64, h * 64:(h + 1) * 64],
                  rhs=onesb[0:64, 0:1], start=True, stop=True)
                for dt in range(4):
                    I("pe", "matmul", R=[Rn("qT"), Rn("Cbf")], W=[Rp_e], out=p_e[0:64, :], lhsT=qT[:, 4 * h + dt, :], rhs=Cbf[:, dt, :],
                      start=(dt == 0), stop=(dt == 3))
                for dt in range(4):
                    I("pe", "matmul", R=[Rn("qT"), Rn("nbf")], W=[Rsm], out=psm[0:64, 196 + h:197 + h], lhsT=qT[:, 4 * h + dt, :],
                      rhs=nbf[:, 4 * h + dt:4 * h + dt + 1], start=(dt == 0), stop=(dt == 3))
                Rd = Rn("dens")
                I("act", "activation", R=[Rsm], W=[Rd], out=dint[0:64, h:h + 1], in_=psm[0:64, 192 + h:193 + h], func=AF.Copy)
                I("dve", "scalar_tensor_tensor", R=[Rsm, Rn("pk"), Rd], W=[Rd], out=den[0:64, h:h + 1], in0=psm[0:64, 196 + h:197 + h],
                  scalar=pk[0:64, h:h + 1], in1=dint[0:64, h:h + 1], op0=ALU.mult, op1=ALU.add)
                I("dve", "tensor_scalar", R=[Rd], W=[Rd], out=nden[0:64, h:h + 1], in0=den[0:64, h:h + 1], scalar1=-1.0, scalar2=None,
                  op0=ALU.mult)
                I("dve", "tensor_tensor", R=[Rd], W=[Rd], out=den[0:64, h:h + 1], in0=den[0:64, h:h + 1], in1=nden[0:64, h:h + 1],
                  op=ALU.max)
                I("dve", "tensor_tensor", R=[Rd, Rn("pk")], W=[Rd], out=den[0:64, h:h + 1], in0=den[0:64, h:h + 1],
                  in1=pk[0:64, 4 + h:5 + h], op=ALU.max)
                I("dve", "reciprocal", R=[Rd], W=[Rd], out=rden[0:64, h:h + 1], in_=den[0:64, h:h + 1])
                I("dve", "tensor_tensor", R=[Rd, Rn("pk")], W=[Rd], out=wr[0:64, h:h + 1], in0=rden[0:64, h:h + 1], in1=pk[0:64, h:h + 1],
                  op=ALU.mult)
                I("act", "activation", R=[Rp_i, Rd], W=[Rn("t1")], out=t1[0:64, :], in_=p_i[0:64, :], func=AF.Copy,
                  scale=rden[0:64, h:h + 1])
                I("dve", "scalar_tensor_tensor", R=[Rp_e, Rd, Rn("t1")], W=[Rn("hh")], out=hh[0:64, :], in0=p_e[0:64, :],
                  scalar=wr[0:64, h:h + 1], in1=t1[0:64, :], op0=ALU.mult, op1=ALU.add)
                I("act", "activation", R=[Rn("hh")], W=[Rn("t1"), Rn("ss")], out=t1[0:64, :], in_=hh[0:64, :], func=AF.Square,
                  accum_out=ss[0:64, h:h + 1])
                I("act", "activation", R=[Rn("ss"), Rc], W=[Rn("rs")], out=rs[0:64, h:h + 1], in_=ss[0:64, h:h + 1], func=AF.Sqrt,
                  scale=1.0 / 512, bias=c[0:64, 128:129])
                I("dve", "reciprocal", R=[Rn("rs")], W=[Rn("rs")], out=rs[0:64, h:h + 1], in_=rs[0:64, h:h + 1])
                I("act", "activation", R=[Rn("hh"), Rn("rs")], W=[Rn("t1")], out=t1[0:64, :], in_=hh[0:64, :], func=AF.Copy,
                  scale=rs[0:64, h:h + 1])
                p_t, Rp_t = self.psum[6], self.R_ps[6]
                for dt in range(4):
                    I("pe", "transpose", R=[Rn("t1"), Rc], W=[Rp_t], out=p_t[:, dt * 64:(dt + 1) * 64],
                      in_=t1[0:64, dt * 128:(dt + 1) * 128], identity=ident[0:64, 0:64])
                fs = slice(4 * h, 4 * h + 4)
                I("pool", "tensor_tensor", R=[Rn("xcT")] + P, W=[Rn("u1")], out=u1, in0=xcT[:, fs, :], in1=self.bc(sk[:, fs], 64),
                  op=ALU.mult)
                I("dve", "tensor_tensor", R=[Rp_t] + P, W=[Rn("u2")], out=u2, in0=p_t[:, 0:256].rearrange("p (j t) -> p j t", j=4),
                  in1=self.bc(ng[:, fs], 64), op=ALU.mult)
                I("pool", "tensor_tensor", R=[Rn("u1"), Rn("u2")], W=[Rn("u2")], out=u2, in0=u2, in1=u1, op=ALU.add)
                I("pool", "tensor_tensor", R=[Rn("u2"), Rn("ogT")], W=[Rn("ogT")], out=ogT[:, fs, :], in0=u2, in1=ogT[:, fs, :],
                  op=ALU.mult)
                for dt in range(4):
                    p_c, Rp_c = self.psum[dt % 2], self.R_ps[dt % 2]
                    I("pe", "matmul", R=[Rn("k_tm"), Rn("v_tm")], W=[Rp_c], out=p_c[:, :],
                      lhsT=k_tm[0:64, (4 * h + dt) * 128:(4 * h + dt + 1) * 128], rhs=v_tm[0:64, h * 512:(h + 1) * 512],
                      start=True, stop=True)
                    I("dve", "scalar_tensor_tensor", R=[Rp_c, Rn("dec_bc"), Rn("Cbf")], W=[Rn("Cst")], out=Cst[:, 4 * h + dt, :],
                      in0=Cst[:, 4 * h + dt, :], scalar=dec_bc[:, h:h + 1], in1=p_c[:, :], op0=ALU.mult, op1=ALU.add)
                    I("pe", "matmul", R=[Rn("k_tm"), Rn("onesb")], W=[Rsm], out=psm[:, 208 + 4 * h + dt:209 + 4 * h + dt],
                      lhsT=k_tm[0:64, (4 * h + dt) * 128:(4 * h + dt + 1) * 128], rhs=onesb[0:64, 0:1], start=True, stop=True)
            I("dve", "tensor_tensor", R=[Rn("dec_bc"), Rn("nbf")], W=[Rn("nst")], out=nst.rearrange("p (h d) -> p h d", h=4),
              in0=nst.rearrange("p (h d) -> p h d", h=4), in1=self.bc(dec_bc, 4), op=ALU.mult)
            I("dve", "tensor_tensor", R=[Rsm], W=[Rn("nst")], out=nst, in0=nst, in1=psm[:, 208:224], op=ALU.add)
            I("act", "activation", R=[Rn("nst")], W=[Rn("nbf")], out=nbf, in_=nst, func=AF.Copy)
            if stage < 5:
                continue
            R_f = Rn("fbuf")
            for ob in range(8):
                pb, Rp = self.psum[4 + ob % 2], self.R_ps[4 + ob % 2]
                for hf in range(2):
                    w, Rw = self.load_w(sc_oout[o, ob, hf], R_scr)
                    for k in range(8):
                        I("pe", "matmul", R=[Rw, Rn("ogT")], W=[Rp], out=pb[:, 0:TM], lhsT=w[:, k, :], rhs=ogT[:, hf * 8 + k, :],
                          start=(hf == 0 and k == 0), stop=(hf == 1 and k == 7))
                I("act", "activation", R=[Rp], W=[R_f], out=self.fbuf[:, ob, 0:TM], in_=pb[:, 0:TM], func=AF.Copy)
            self.epilogue(None, self.lnw["ln_mix_post"], l, t0=t0, n=TM)


def make_consts():
    c = np.zeros((128, 2048), np.float32)
    c[:, 0:128] = np.eye(128, dtype=np.float32)
    c[:, 128] = EPS
    c[:, 129] = np.pi / 2
    c[:, 130] = 1.0
    p = np.arange(128)
    for g in range(8):
        c[:, 136 + g] = (p // 16 == g)
    for g in range(4):
        c[:, 144 + g] = (p % 4 == g)
    j = np.arange(64)[:, None]
    i = np.arange(64)[None, :]
    c[0:64, 256:320] = (j <= i)
    c[64:128, 256:320] = (j <= i)
    c[0:64, 320:384] = (j <= i)
    c[:, 512:1024] = np.arange(512, dtype=np.float32)[None, :]
    cm = np.ones(512, np.float32)
    cm[::64] = 0
    c[:, 1024:1536] = cm[None, :]
    cc = np.arange(128)
    c[:, 384:512] = (p[:, None] // 4 == cc[None, :] // 4)
    for h in range(4):
        c[h, 1536 + h * 64:1536 + (h + 1) * 64] = 1.0
    c[:, 1792:1984] = 1.0
    return c


_CACHE = {}


def run_module(inputs, nseq, layers, parts, batch_sel=None):
    key = (nseq, tuple(layers), tuple(parts))
    if key not in _CACHE:
        b = Builder(nseq, layers, parts)
        _CACHE[key] = (b.build(), list(b.drams.keys()))
    nc, nc_inputs = _CACHE[key]
    x = np.asarray(inputs["x"], dtype=np.float32)
    in_maps = []
    for c in range(NCORES):
        m = {}
        if batch_sel is None:
            m["x"] = np.ascontiguousarray(x[c * nseq:(c + 1) * nseq])
        else:
            m["x"] = np.ascontiguousarray(x[batch_sel[c]])
        for k in nc_inputs:
            if k not in ("x", "consts"):
                m[k] = np.ascontiguousarray(np.asarray(inputs[k], dtype=np.float32))
        m["consts"] = make_consts()
        in_maps.append(m)
    res = run_bass_kernel_spmd(nc, in_maps, core_ids=list(range(NCORES)))
    return np.concatenate([r["y"] for r in res.results], axis=0)


def kernel(**inputs):
    mode = MODE
    x = np.asarray(inputs["x"], dtype=np.float32)
    if mode == "fused":
        return run_module(inputs, NB // NCORES, list(range(DEPTH)), ("mix", "ffn")).astype(np.float32)
    out = np.empty_like(x)
    per = NB // NCORES
    for s_ in range(per):
        sel = [[c * per + s_] for c in range(NCORES)]
        y = run_module(inputs, 1, list(range(DEPTH)), ("mix", "ffn"), batch_sel=sel)
        for c in range(NCORES):
            out[c * per + s_] = y[c]
    return out


MODE = "fused"
```
